# Optimizing a Trainium2 kernel written in Bass

```python
import math
import jax, jax.numpy as jnp
from jax import lax
import numpy as np

D_MODEL = 1024
BATCH = 16
SEQ = 2048
DEPTH = 2

HEAD_DIM = 64
Q_BLOCK = 128
ROPE_THETA = 10000.0
LN_EPS = 1e-5
RMS_EPS = 1e-6
MLA_HEADS = 4
MLA_Q_LORA = 256
MLA_KV_LORA = 128
MLA_NOPE = 64
MLA_ROPE = 32
MLA_V = 64
SWA_HEADS = 4
SWA_KV_HEADS = 2
SWA_WINDOW = 128
NSA_HEADS = 8
NSA_KV_HEADS = 2
NSA_BLOCK = 32
NSA_TOP_N = 8
NSA_WINDOW = 128
NSA_PHI_HIDDEN = 256
N_BRANCH = 3
FORCE_SCORE = 1e4
D_MIX = MLA_HEADS * MLA_V + SWA_HEADS * HEAD_DIM + NSA_HEADS * HEAD_DIM
IN_SIZES = (MLA_Q_LORA, MLA_KV_LORA, MLA_ROPE,
            SWA_HEADS * HEAD_DIM, SWA_KV_HEADS * HEAD_DIM, SWA_KV_HEADS * HEAD_DIM,
            NSA_HEADS * HEAD_DIM,
            NSA_KV_HEADS * HEAD_DIM, NSA_KV_HEADS * HEAD_DIM,
            NSA_KV_HEADS * HEAD_DIM, NSA_KV_HEADS * HEAD_DIM,
            NSA_KV_HEADS * HEAD_DIM, NSA_KV_HEADS * HEAD_DIM,
            NSA_HEADS * N_BRANCH)
D_IN = sum(IN_SIZES)
N_EXPERTS = 16
N_GROUPS = 4
EXPERTS_PER_GROUP = N_EXPERTS // N_GROUPS
TOP_K = 2
D_EXPERT = 256
ALPHA = (2 * DEPTH) ** 0.25
BETA = (8 * DEPTH) ** -0.25
MAX_POS_OFFSET = 4096

kernel_name = "hybrid_mla_swa_nsa_moe_deepnorm_adaln"


def _ln(x):
    xf = x.astype(jnp.float32)
    mu = jnp.mean(xf, -1, keepdims=True)
    var = jnp.mean(jnp.square(xf - mu), -1, keepdims=True)
    return (xf - mu) * lax.rsqrt(var + LN_EPS)


def layernorm(x, g, b):
    return (_ln(x) * g + b).astype(x.dtype)


def rmsnorm(x, g):
    xf = x.astype(jnp.float32)
    y = xf * lax.rsqrt(jnp.mean(jnp.square(xf), -1, keepdims=True) + RMS_EPS)
    return (y * g).astype(x.dtype)


def rope_tables(positions, dim):
    inv_freq = 1.0 / (ROPE_THETA ** (jnp.arange(0, dim, 2, dtype=jnp.float32) / dim))
    ang = positions.astype(jnp.float32)[..., None] * inv_freq
    return jnp.cos(ang)[:, :, None, :], jnp.sin(ang)[:, :, None, :]


def apply_rope(x, cos, sin):
    xf = x.astype(jnp.float32)
    x1, x2 = jnp.split(xf, 2, axis=-1)
    return jnp.concatenate([x1 * cos - x2 * sin, x2 * cos + x1 * sin], -1).astype(x.dtype)


def masked_softmax(s, mask):
    s = jnp.where(mask, s, -jnp.inf)
    m = jnp.max(s, axis=-1, keepdims=True)
    m = jnp.where(jnp.isfinite(m), m, 0.0)
    e = jnp.exp(s - m)
    den = jnp.sum(e, axis=-1, keepdims=True)
    return e / jnp.where(den > 0, den, 1.0)


def to_blocks(x):
    B, T = x.shape[:2]
    return jnp.moveaxis(x.reshape(B, T // Q_BLOCK, Q_BLOCK, *x.shape[2:]), 1, 0)


def from_blocks(y):
    NQ, B, Q = y.shape[:3]
    return jnp.moveaxis(y, 0, 1).reshape(B, NQ * Q, *y.shape[3:])


def mla_attention(c_q, c_kv, k_pe, cos_r, sin_r, q_norm, w_uq, kv_norm, w_ukv):
    B, T, _ = c_q.shape
    q = (rmsnorm(c_q, q_norm) @ w_uq).reshape(B, T, MLA_HEADS, MLA_NOPE + MLA_ROPE)
    q_nope = q[..., :MLA_NOPE]
    q_pe = apply_rope(q[..., MLA_NOPE:], cos_r, sin_r)
    kv = (rmsnorm(c_kv, kv_norm) @ w_ukv).reshape(B, T, MLA_HEADS, MLA_NOPE + MLA_V)
    k_nope, v = kv[..., :MLA_NOPE], kv[..., MLA_NOPE:]
    k_pe = apply_rope(k_pe[:, :, None, :], cos_r, sin_r)[:, :, 0]
    scale = 1.0 / math.sqrt(MLA_NOPE + MLA_ROPE)
    kpos = jnp.arange(T)

    def block(args):
        qn, qp, q0 = args
        s = (jnp.einsum('bqhd,bkhd->bhqk', qn, k_nope)
             + jnp.einsum('bqhd,bkd->bhqk', qp, k_pe)).astype(jnp.float32) * scale
        qpos = q0 + jnp.arange(Q_BLOCK)
        p = masked_softmax(s, kpos[None, :] <= qpos[:, None])
        return jnp.einsum('bhqk,bkhd->bqhd', p.astype(v.dtype), v)

    starts = jnp.arange(T // Q_BLOCK, dtype=jnp.int32) * Q_BLOCK
    o = lax.map(block, (to_blocks(q_nope), to_blocks(q_pe), starts))
    return from_blocks(o).reshape(B, T, MLA_HEADS * MLA_V)


def banded_window_attention(q, k, v, window, sinks=None):
    B, T, Hq, d = q.shape
    Hkv = k.shape[2]
    R = Hq // Hkv
    NB = T // Q_BLOCK
    n_prev = -(-window // Q_BLOCK)
    L = (n_prev + 1) * Q_BLOCK
    pad = ((0, 0), (n_prev * Q_BLOCK, 0), (0, 0), (0, 0))
    kp = jnp.pad(k, pad).reshape(B, NB + n_prev, Q_BLOCK, Hkv, d)
    vp = jnp.pad(v, pad).reshape(B, NB + n_prev, Q_BLOCK, Hkv, d)
    kb = jnp.concatenate([kp[:, i:i + NB] for i in range(n_prev + 1)], axis=2)
    vb = jnp.concatenate([vp[:, i:i + NB] for i in range(n_prev + 1)], axis=2)
    qb = q.reshape(B, NB, Q_BLOCK, Hkv, R, d)
    s = jnp.einsum('bnqgrd,bnkgd->bngrqk', qb, kb).astype(jnp.float32) / math.sqrt(d)
    qi = jnp.arange(Q_BLOCK)[:, None]
    ki = jnp.arange(L)[None, :]
    rel = qi + n_prev * Q_BLOCK - ki
    kpos = jnp.arange(NB)[:, None, None] * Q_BLOCK - n_prev * Q_BLOCK + ki[None]
    mask = (((rel >= 0) & (rel < window))[None] & (kpos >= 0))[None, :, None, None]
    if sinks is None:
        p = masked_softmax(s, mask)
    else:
        sink = sinks.astype(jnp.float32).reshape(1, 1, Hkv, R, 1, 1)
        s = jnp.where(mask, s, -jnp.inf)
        m = jnp.maximum(jnp.max(s, -1, keepdims=True), sink)
        e = jnp.exp(s - m)
        p = e / (jnp.sum(e, -1, keepdims=True) + jnp.exp(sink - m))
    o = jnp.einsum('bngrqk,bnkgd->bnqgrd', p.astype(v.dtype), vb)
    return o.reshape(B, T, Hq * d)


def nsa_attention(q, kc_raw, vc_raw, ks, vs, kw, vw, gate_logits,
                  cmp_pos, phi_k1, phi_k2, phi_v1, phi_v2):
    B, T, H, d = q.shape
    G = NSA_KV_HEADS
    R = H // G
    NC = T // NSA_BLOCK
    scale = 1.0 / math.sqrt(d)
    qg = q.reshape(B, T, G, R, d)

    def compress(z, w1, w2):
        zb = z.reshape(B, NC, NSA_BLOCK, G, d) + cmp_pos[None, None, :, None, :]
        zb = jnp.moveaxis(zb, 3, 2).reshape(B, NC, G, NSA_BLOCK * d)
        return jax.nn.gelu(zb @ w1) @ w2

    k_cmp = compress(kc_raw, phi_k1, phi_k2)
    v_cmp = compress(vc_raw, phi_v1, phi_v2)

    t = jnp.arange(T)
    j = jnp.arange(NC)
    cmp_mask = (j[None, :] * NSA_BLOCK + NSA_BLOCK - 1) <= t[:, None]
    s_cmp = jnp.einsum('btgrd,bcgd->bgrtc', qg, k_cmp).astype(jnp.float32) * scale
    p_cmp = masked_softmax(s_cmp, cmp_mask)
    o_cmp = jnp.einsum('bgrtc,bcgd->btgrd', p_cmp.astype(v_cmp.dtype), v_cmp)

    imp = jnp.sum(p_cmp, axis=2)
    tb = t // NSA_BLOCK
    future = j[None, :] * NSA_BLOCK > t[:, None]
    forced = (j[None, :] == 0) | (j[None, :] == tb[:, None]) | (j[None, :] == tb[:, None] - 1)
    imp = jnp.where(future, -jnp.inf, jnp.where(forced, FORCE_SCORE, imp))
    n_sel = min(NSA_TOP_N, NC)
    _, sel = lax.top_k(imp, n_sel)

    k_blk = jnp.moveaxis(ks.reshape(B, NC, NSA_BLOCK, G, d), 3, 1)
    v_blk = jnp.moveaxis(vs.reshape(B, NC, NSA_BLOCK, G, d), 3, 1)
    gather = jax.vmap(jax.vmap(lambda tbl, idx: tbl[idx]))
    M = n_sel * NSA_BLOCK
    sel_b = jnp.moveaxis(sel.reshape(B, G, T // Q_BLOCK, Q_BLOCK, n_sel), 2, 0)
    starts = jnp.arange(T // Q_BLOCK, dtype=jnp.int32) * Q_BLOCK

    def block(args):
        qc, idx, q0 = args
        kg = gather(k_blk, idx).reshape(B, G, Q_BLOCK, M, d)
        vg = gather(v_blk, idx).reshape(B, G, Q_BLOCK, M, d)
        s = jnp.einsum('bqgrd,bgqmd->bgrqm', qc, kg).astype(jnp.float32) * scale
        kpos = (idx[..., None] * NSA_BLOCK + jnp.arange(NSA_BLOCK)).reshape(B, G, Q_BLOCK, M)
        qpos = q0 + jnp.arange(Q_BLOCK)
        mask = (kpos <= qpos[None, None, :, None])[:, :, None]
        p = masked_softmax(s, mask)
        return jnp.einsum('bgrqm,bgqmd->bqgrd', p.astype(vg.dtype), vg)

    o_slc = from_blocks(lax.map(block, (to_blocks(qg), sel_b, starts)))
    o_win = banded_window_attention(q, kw, vw, NSA_WINDOW).reshape(B, T, H, d)
    g = jax.nn.sigmoid(gate_logits.astype(jnp.float32)).astype(q.dtype)
    o = (g[..., 0:1] * o_cmp.reshape(B, T, H, d)
         + g[..., 1:2] * o_slc.reshape(B, T, H, d)
         + g[..., 2:3] * o_win)
    return o.reshape(B, T, H * d)


def moe(h, router_w, router_bias, w_gate, w_up, w_down):
    B, T, D = h.shape
    hf = h.reshape(B * T, D)
    scores = jax.nn.sigmoid((hf @ router_w).astype(jnp.float32))
    biased = scores + router_bias.astype(jnp.float32)
    grouped = biased.reshape(-1, N_GROUPS, EXPERTS_PER_GROUP)
    group_score = jnp.sum(lax.top_k(grouped, TOP_K)[0], -1)
    grp = jnp.argmax(group_score, -1)
    in_grp = jnp.arange(N_GROUPS)[None, :] == grp[:, None]
    masked = jnp.where(in_grp[:, :, None], grouped, -jnp.inf).reshape(-1, N_EXPERTS)
    _, eidx = lax.top_k(masked, TOP_K)
    w = jnp.take_along_axis(scores, eidx, axis=1)
    w = w / jnp.sum(w, -1, keepdims=True)
    combine = jnp.sum(jax.nn.one_hot(eidx, N_EXPERTS, dtype=jnp.float32) * w[..., None], axis=1)
    g = jnp.einsum('nd,edf->nef', hf, w_gate)
    u = jnp.einsum('nd,edf->nef', hf, w_up)
    a = jax.nn.silu(g) * u * combine[:, :, None].astype(h.dtype)
    y = jnp.einsum('nef,efd->nd', a, w_down)
    return y.reshape(B, T, D)


def setup_inputs(seed: int = 0) -> dict:
    key = jax.random.key(seed)
    ks = jax.random.split(key, 32)
    f32 = jnp.float32

    def nrm(k, shape, s):
        return jax.random.normal(k, shape, f32) * s

    offs = jax.random.randint(ks[2], (BATCH, 1), 0, MAX_POS_OFFSET, dtype=jnp.int32)
    positions = jnp.arange(SEQ, dtype=jnp.int32)[None, :] + offs
    return {
        "x": nrm(ks[0], (BATCH, SEQ, D_MODEL), 1.0),
        "c": nrm(ks[1], (BATCH, D_MODEL), 1.0),
        "positions": positions,
        "ada_w": nrm(ks[3], (DEPTH, D_MODEL, 6 * D_MODEL), 0.1 * D_MODEL ** -0.5),
        "ada_b": nrm(ks[4], (DEPTH, 6 * D_MODEL), 0.02),
        "w_in": nrm(ks[5], (DEPTH, D_MODEL, D_IN), D_MODEL ** -0.5),
        "mla_q_norm": 1.0 + nrm(ks[6], (DEPTH, MLA_Q_LORA), 0.02),
        "mla_w_uq": nrm(ks[7], (DEPTH, MLA_Q_LORA, MLA_HEADS * (MLA_NOPE + MLA_ROPE)), MLA_Q_LORA ** -0.5),
        "mla_kv_norm": 1.0 + nrm(ks[8], (DEPTH, MLA_KV_LORA), 0.02),
        "mla_w_ukv": nrm(ks[9], (DEPTH, MLA_KV_LORA, MLA_HEADS * (MLA_NOPE + MLA_V)), MLA_KV_LORA ** -0.5),
        "swa_sinks": nrm(ks[10], (DEPTH, SWA_HEADS), 1.0),
        "nsa_cmp_pos": nrm(ks[11], (DEPTH, NSA_BLOCK, HEAD_DIM), 0.1),
        "nsa_phi_k1": nrm(ks[12], (DEPTH, NSA_BLOCK * HEAD_DIM, NSA_PHI_HIDDEN), (NSA_BLOCK * HEAD_DIM) ** -0.5),
        "nsa_phi_k2": nrm(ks[13], (DEPTH, NSA_PHI_HIDDEN, HEAD_DIM), NSA_PHI_HIDDEN ** -0.5),
        "nsa_phi_v1": nrm(ks[14], (DEPTH, NSA_BLOCK * HEAD_DIM, NSA_PHI_HIDDEN), (NSA_BLOCK * HEAD_DIM) ** -0.5),
        "nsa_phi_v2": nrm(ks[15], (DEPTH, NSA_PHI_HIDDEN, HEAD_DIM), NSA_PHI_HIDDEN ** -0.5),
        "w_out": nrm(ks[16], (DEPTH, D_MIX, D_MODEL), BETA * D_MIX ** -0.5),
        "ln1_g": 1.0 + nrm(ks[17], (DEPTH, D_MODEL), 0.02),
        "ln1_b": nrm(ks[18], (DEPTH, D_MODEL), 0.02),
        "ln2_g": 1.0 + nrm(ks[19], (DEPTH, D_MODEL), 0.02),
        "ln2_b": nrm(ks[20], (DEPTH, D_MODEL), 0.02),
        "router_w": nrm(ks[21], (D_MODEL, N_EXPERTS), D_MODEL ** -0.5),
        "router_bias": nrm(ks[22], (N_EXPERTS,), 0.01),
        "moe_w_gate": nrm(ks[23], (DEPTH, N_EXPERTS, D_MODEL, D_EXPERT), D_MODEL ** -0.5),
        "moe_w_up": nrm(ks[24], (DEPTH, N_EXPERTS, D_MODEL, D_EXPERT), BETA * D_MODEL ** -0.5),
        "moe_w_down": nrm(ks[25], (DEPTH, N_EXPERTS, D_EXPERT, D_MODEL), BETA * D_EXPERT ** -0.5),
    }


def reference(x, c, positions, ada_w, ada_b, w_in, mla_q_norm, mla_w_uq, mla_kv_norm, mla_w_ukv,
              swa_sinks, nsa_cmp_pos, nsa_phi_k1, nsa_phi_k2, nsa_phi_v1, nsa_phi_v2, w_out,
              ln1_g, ln1_b, ln2_g, ln2_b, router_w, router_bias, moe_w_gate, moe_w_up, moe_w_down):
    B, T, D = x.shape
    cos_h, sin_h = rope_tables(positions, HEAD_DIM)
    cos_r, sin_r = rope_tables(positions, MLA_ROPE)
    cond = jax.nn.silu(c)
    split_at = np.cumsum(IN_SIZES)[:-1].tolist()

    def heads(z, n):
        return z.reshape(B, T, n, -1)

    for l in range(DEPTH):
        mod = cond @ ada_w[l] + ada_b[l]
        sh1, sc1, gt1, sh2, sc2, gt2 = jnp.split(mod[:, None, :], 6, axis=-1)

        h = (_ln(x) * (1.0 + sc1) + sh1).astype(x.dtype)
        (cq, ckv, kpe, sq, sk, sv, nq, nkc, nvc, nks, nvs, nkw, nvw, ngate) = jnp.split(
            h @ w_in[l], split_at, axis=-1)
        o_a = mla_attention(cq, ckv, kpe, cos_r, sin_r,
                            mla_q_norm[l], mla_w_uq[l], mla_kv_norm[l], mla_w_ukv[l])
        o_b = banded_window_attention(apply_rope(heads(sq, SWA_HEADS), cos_h, sin_h),
                                      apply_rope(heads(sk, SWA_KV_HEADS), cos_h, sin_h),
                                      heads(sv, SWA_KV_HEADS), SWA_WINDOW, swa_sinks[l])
        o_c = nsa_attention(apply_rope(heads(nq, NSA_HEADS), cos_h, sin_h),
                            apply_rope(heads(nkc, NSA_KV_HEADS), cos_h, sin_h), heads(nvc, NSA_KV_HEADS),
                            apply_rope(heads(nks, NSA_KV_HEADS), cos_h, sin_h), heads(nvs, NSA_KV_HEADS),
                            apply_rope(heads(nkw, NSA_KV_HEADS), cos_h, sin_h), heads(nvw, NSA_KV_HEADS),
                            heads(ngate, NSA_HEADS),
                            nsa_cmp_pos[l], nsa_phi_k1[l], nsa_phi_k2[l], nsa_phi_v1[l], nsa_phi_v2[l])
        y = jnp.concatenate([o_a, o_b, o_c], axis=-1) @ w_out[l]
        x = layernorm(ALPHA * x + (1.0 + gt1) * y, ln1_g[l], ln1_b[l])

        h = (_ln(x) * (1.0 + sc2) + sh2).astype(x.dtype)
        y = moe(h, router_w, router_bias, moe_w_gate[l], moe_w_up[l], moe_w_down[l])
        x = layernorm(ALPHA * x + (1.0 + gt2) * y, ln2_g[l], ln2_b[l])
    return x
```

```python
from contextlib import ExitStack
import math
import numpy as np
import concourse.bass as bass
import concourse.mybir as mybir
from concourse.bass_utils import run_bass_kernel_spmd

F32 = mybir.dt.float32
BF16 = mybir.dt.bfloat16
I32 = mybir.dt.int32
ALU = mybir.AluOpType
AF = mybir.ActivationFunctionType
AX = mybir.AxisListType

T_ = 2048
D_ = 1024
NT = 16
DEPTH = 2
ALPHA = (2 * DEPTH) ** 0.25
LN_EPS = 1e-5
RMS_EPS = 1e-6
NEG = -30000.0
SEM_EPOCH = 12000
MAX_INFLIGHT_DMA = 8
LNW = 4

O_CQ, O_CKV, O_KPE, O_SQ, O_SK, O_SV, O_NQ, O_NKC, O_NVC, O_NKS, O_NVS, O_NKW, O_NVW, O_NG = (
    0, 256, 384, 416, 672, 800, 928, 1440, 1568, 1696, 1824, 1952, 2080, 2208)


class Chan:
    __slots__ = ("sem", "count", "step", "sig", "q", "alt")

    def __init__(self, sem, step):
        self.sem = sem
        self.count = 0
        self.step = step
        self.sig = None
        self.q = None
        self.alt = None


class Buf:
    __slots__ = ("name", "w", "r", "ch")

    def __init__(self, name):
        self.name = name
        self.w = None
        self.r = {}
        self.ch = None


class Em:
    ENGS = ("pe", "act", "dve", "pool", "sp")

    def __init__(self, nc, stack):
        self.nc = nc
        self.stack = stack
        self.prog = {e: [] for e in self.ENGS}
        self.known = {e: {} for e in self.ENGS}
        self.nsem = 0
        self.all_chans = []
        self.free_dma = []
        self.live_dma = []
        self.inflight = {e: [] for e in self.ENGS}
        self.chan = {}
        for e in ("pe", "act", "dve", "pool"):
            self.chan[e] = self.new_chan(1, e)
        self.ninst = 0

    def _raw_dma_chan(self, name):
        self.nsem += 1
        sem = self.stack.enter_context(self.nc.semaphore(f"s{self.nsem}_{name}"))
        ch = Chan(sem, 16)
        self.all_chans.append(ch)
        return ch

    def new_chan(self, step=16, name="c"):
        if step == 16:
            if self.free_dma:
                ch = self.free_dma.pop()
            else:
                ch = self._raw_dma_chan(name)
            self.live_dma.append(ch)
            return ch
        self.nsem += 1
        sem = self.stack.enter_context(self.nc.semaphore(f"s{self.nsem}_{name}"))
        ch = Chan(sem, step)
        self.all_chans.append(ch)
        return ch

    def _need(self, eng, waits, dep):
        if dep is None:
            return
        ch, val = dep
        if eng == "pe" and ch is self.chan.get("pe"):
            return
        if self.known[eng].get(ch, 0) >= val:
            return
        if waits.get(ch, 0) < val:
            waits[ch] = val

    def _deps(self, eng, reads, writes, same_ch=None):
        waits = {}
        for b in reads:
            self._need(eng, waits, b.w)
        for b in writes:
            if same_ch is not None and b.w is not None and b.w[0] is same_ch and not b.r:
                continue
            self._need(eng, waits, b.w)
            for ch, val in b.r.items():
                self._need(eng, waits, (ch, val))
        for ch, val in waits.items():
            self.known[eng][ch] = val
        return list(waits.items())

    def _commit(self, ch, reads, writes):
        ch.count += ch.step
        val = ch.count
        for b in reads:
            if b.r.get(ch, 0) < val:
                b.r[ch] = val
        for b in writes:
            b.w = (ch, val)
            b.r = {}

    def op(self, eng, fn, reads=(), writes=()):
        if self.chan[eng].count >= SEM_EPOCH:
            self.chan[eng] = self.new_chan(1, eng)
        reads = [x.b if hasattr(x, "b") else x for x in reads]
        writes = [x.b if hasattr(x, "b") else x for x in writes]
        waits = self._deps(eng, reads, writes)
        ch = self.chan[eng]
        self._commit(ch, reads, writes)
        self.prog[eng].append((waits, fn, ch, ch.count))
        self.ninst += 1

    def dma(self, q, ch, fn, reads=(), writes=()):
        reads = [x.b if hasattr(x, "b") else x for x in reads]
        writes = [x.b if hasattr(x, "b") else x for x in writes]
        if ch.q is None:
            ch.q = q
        elif ch.q != q:
            if ch.alt is None:
                ch.alt = self._raw_dma_chan("alt")
                ch.alt.q = q
            ch = ch.alt
        waits = self._deps(q, reads, writes, same_ch=ch)
        sig = frozenset(id(b) for b in list(reads) + list(writes))
        if ch.count > 0 and ch.sig != sig and self.known[q].get(ch, 0) < ch.count:
            waits = [w for w in waits if w[0] is not ch] + [(ch, ch.count)]
            self.known[q][ch] = ch.count
        ch.sig = sig
        fifo = self.inflight[q]
        fifo[:] = [(c_, v_) for (c_, v_) in fifo if self.known[q].get(c_, 0) < v_]
        while len(fifo) >= MAX_INFLIGHT_DMA:
            c_, v_ = fifo.pop(0)
            if self.known[q].get(c_, 0) < v_:
                waits = [w for w in waits if not (w[0] is c_ and w[1] <= v_)] + [(c_, v_)]
                self.known[q][c_] = v_
            fifo[:] = [(c2, v2) for (c2, v2) in fifo if self.known[q].get(c2, 0) < v2]
        self._commit(ch, reads, writes)
        fifo.append((ch, ch.count))
        self.prog[q].append((waits, fn, ch, ch.count))
        self.ninst += 1

    def chan_mark(self):
        return len(self.live_dma)

    def chan_release(self, tok):
        rel = self.live_dma[tok:]
        del self.live_dma[tok:]
        self.free_dma.extend(rel)

    def barrier(self):
        for eng in self.ENGS:
            waits = []
            for ch in self.all_chans:
                if ch.count > 0 and self.known[eng].get(ch, 0) < ch.count:
                    if eng == "pe" and ch is self.chan["pe"]:
                        continue
                    waits.append((ch, ch.count))
                    self.known[eng][ch] = ch.count
            if waits:
                self.prog[eng].append((waits, None, None, 0))

    def emit(self):
        nc = self.nc
        prog = self.prog
        targets = {}
        for eng in self.ENGS:
            for waits, fn, ch, val in prog[eng]:
                for wch, wval in waits:
                    if wch.step == 1:
                        targets.setdefault(wch, set()).add(wval)
        remap = {ch: {v: i + 1 for i, v in enumerate(sorted(vals))} for ch, vals in targets.items()}
        self.n_inc = 0
        with nc.Block() as block:
            def run(engh, items):
                for waits, fn, ch, val in items:
                    for wch, wval in waits:
                        if wch.step == 1:
                            engh.wait_ge(wch.sem, remap[wch][wval])
                        else:
                            engh.wait_ge(wch.sem, wval)
                    if fn is not None:
                        ins = fn(engh)
                        if ch.step == 16:
                            ins.then_inc(ch.sem, 16)
                        elif val in remap.get(ch, ()):
                            ins.then_inc(ch.sem, 1)
                            self.n_inc += 1

            @block.tensor
            def _(e):
                run(e, prog["pe"])

            @block.scalar
            def _(e):
                run(e, prog["act"])

            @block.vector
            def _(e):
                run(e, prog["dve"])

            @block.gpsimd
            def _(e):
                run(e, prog["pool"])

            @block.sync
            def _(e):
                run(e, prog["sp"])


class TT:
    def __init__(self, em, stack, name, shape, dtype, psum=False):
        nc = em.nc
        if psum:
            self.t = stack.enter_context(nc.psum_tensor(name, list(shape), dtype))
        else:
            self.t = stack.enter_context(nc.sbuf_tensor(name, list(shape), dtype))
        self.b = Buf(name)

    def __getitem__(self, k):
        return self.t[k]


class Rot:
    def __init__(self, items):
        self.items = items
        self.i = 0

    def next(self):
        x = self.items[self.i % len(self.items)]
        self.i += 1
        return x


def build(n_seq=2, n_layers=2, dbg=False, upto=99):
    nc = bass.Bass("TRN2", target_bir_lowering=False)

    def din(name, shape, dt=F32):
        return nc.dram_tensor(name, list(shape), dt, kind="ExternalInput").ap()

    x_in = din("x", [n_seq, T_, D_])
    c_in = din("c", [n_seq, D_])
    pos_in = din("positions", [n_seq, T_], I32)
    ada_w = din("ada_w", [2, 1024, 6144])
    ada_b = din("ada_b", [2, 6144])
    w_in = din("w_in", [2, 1024, 2232])
    mla_qn = din("mla_q_norm", [2, 256])
    mla_wuq = din("mla_w_uq", [2, 256, 384])
    mla_kvn = din("mla_kv_norm", [2, 128])
    mla_wukv = din("mla_w_ukv", [2, 128, 512])
    swa_sinks = din("swa_sinks", [2, 4])
    cmp_pos = din("nsa_cmp_pos", [2, 32, 64])
    phi_k1 = din("nsa_phi_k1", [2, 2048, 256])
    phi_k2 = din("nsa_phi_k2", [2, 256, 64])
    phi_v1 = din("nsa_phi_v1", [2, 2048, 256])
    phi_v2 = din("nsa_phi_v2", [2, 256, 64])
    w_out = din("w_out", [2, 1024, 1024])
    ln1_g = din("ln1_g", [2, 1024])
    ln1_b = din("ln1_b", [2, 1024])
    ln2_g = din("ln2_g", [2, 1024])
    ln2_b = din("ln2_b", [2, 1024])
    router_w = din("router_w", [1024, 16])
    router_bias = din("router_bias", [16])
    moe_wg = din("moe_w_gate", [2, 16, 1024, 256])
    moe_wu = din("moe_w_up", [2, 16, 1024, 256])
    moe_wd = din("moe_w_down", [2, 16, 256, 1024])
    k_ident = din("k_ident", [128, 128])
    k_invf = din("k_invf", [128, 2])
    k_sgn = din("k_sgn", [128, 2])
    k_mdiag4 = din("k_mdiag4", [128, 512])
    k_mprev4 = din("k_mprev4", [128, 512])
    k_mwin4 = din("k_mwin4", [128, 512])
    k_cmpmask = din("k_cmpmask", [128, NT * 256])
    k_keep = din("k_keep", [128, NT * 64])
    k_add = din("k_add", [128, NT * 64])
    k_expand = din("k_expand", [128, 2048])
    k_sele = din("k_sele", [128, 2048])

    out = nc.dram_tensor("out", [n_seq, T_, D_], F32, kind="ExternalOutput").ap()
    xa = nc.dram_tensor("xa", [n_seq, T_, D_], F32).ap()
    xb = nc.dram_tensor("xb", [n_seq, T_, D_], F32).ap()
    o_scr = nc.dram_tensor("o_scr", [n_seq, T_, D_], BF16).ap()
    tab_scr = nc.dram_tensor("tab_scr", [n_seq, 4, 128, T_], F32).ap()
    dbg_o = None
    if dbg:
        dbg_o = nc.dram_tensor("dbg_o", [n_seq, T_, D_], BF16, kind="ExternalOutput").ap()
        dbg_x1 = nc.dram_tensor("dbg_x1", [n_seq, T_, D_], F32, kind="ExternalOutput").ap()

    with ExitStack() as gs:
        em = Em(nc, gs)
        dram_bufs = {}

        def dbuf(name):
            if name not in dram_bufs:
                dram_bufs[name] = Buf(name)
            return dram_bufs[name]

        uniq = [0]

        def S(stack, name, shape, dt=F32):
            uniq[0] += 1
            return TT(em, stack, f"{name}_{uniq[0]}", shape, dt)

        def mm(out_, lhsT, rhs, start, stop, R, W):
            em.op("pe", lambda e: e.matmul(out_, lhsT=lhsT, rhs=rhs, start=start, stop=stop), R, W)

        def tr(out_, in_, R, W, ident):
            em.op("pe", lambda e: e.transpose(out=out_, in_=in_, identity=ident), R, W)

        def act(out_, in_, func, R, W, bias=0.0, scale=1.0, accum=None):
            if accum is None:
                em.op("act", lambda e: e.activation(out=out_, in_=in_, func=func, bias=bias, scale=scale), R, W)
            else:
                em.op("act", lambda e: e.activation(out=out_, in_=in_, func=func, bias=bias, scale=scale,
                                                    accum_out=accum), R, W)

        def cp(eng, out_, in_, R, W):
            if eng == "act":
                em.op("act", lambda e: e.copy(out=out_, in_=in_), R, W)
            else:
                em.op(eng, lambda e: e.tensor_copy(out=out_, in_=in_), R, W)

        def tt(eng, out_, a, b, op, R, W):
            em.op(eng, lambda e: e.tensor_tensor(out=out_, in0=a, in1=b, op=op), R, W)

        def ts(eng, out_, a, s1, s2, op0, op1, R, W):
            if s2 is None:
                em.op(eng, lambda e: e.tensor_scalar(out=out_, in0=a, scalar1=s1, scalar2=None, op0=op0), R, W)
            else:
                em.op(eng, lambda e: e.tensor_scalar(out=out_, in0=a, scalar1=s1, scalar2=s2, op0=op0, op1=op1), R, W)

        def stt(out_, in0, scalar, in1, op0, op1, R, W):
            em.op("dve", lambda e: e.scalar_tensor_tensor(out=out_, in0=in0, scalar=scalar, in1=in1, op0=op0, op1=op1), R, W)

        def red(out_, in_, op, R, W, axis=AX.X):
            em.op("dve", lambda e: e.tensor_reduce(out=out_, in_=in_, axis=axis, op=op), R, W)

        def recip(out_, in_, R, W):
            em.op("dve", lambda e: e.reciprocal(out=out_, in_=in_), R, W)

        def memset(eng, out_, val, W):
            em.op(eng, lambda e: e.memset(out_, val), (), W)

        def load(q, dst_t, dst_ap, src_ap, ch, src_buf=None, slow=False):
            if ch is None:
                if dst_t.b.ch is None:
                    dst_t.b.ch = em.new_chan(16, "ld")
                ch = dst_t.b.ch
            R = [src_buf] if src_buf is not None else []
            if slow:
                em.dma(q, ch, lambda e: e.dma_start(out=dst_ap, in_=src_ap, allow_slow_non_contiguous=True), R, [dst_t])
            else:
                em.dma(q, ch, lambda e: e.dma_start(out=dst_ap, in_=src_ap), R, [dst_t])

        def store(q, dst_ap, dst_buf, src_t, src_ap, ch):
            em.dma(q, ch, lambda e: e.dma_start(out=dst_ap, in_=src_ap), [src_t], [dst_buf])

        identb = S(gs, "identb", [128, 128], BF16)
        onesb = S(gs, "onesb", [128, 128], BF16)
        zerosb = S(gs, "zerosb", [128, 512], BF16)
        mdiag4 = S(gs, "mdiag4", [128, 512], BF16)
        mprev4 = S(gs, "mprev4", [128, 512], BF16)
        mwin4 = S(gs, "mwin4", [128, 512], BF16)
        invf = S(gs, "invf", [128, 2])
        sgn = S(gs, "sgn", [128, 2])
        cch = None
        load("pool", identb, identb[:], k_ident, cch)
        load("pool", mdiag4, mdiag4[:], k_mdiag4, cch)
        load("pool", mprev4, mprev4[:], k_mprev4, cch)
        load("pool", mwin4, mwin4[:], k_mwin4, cch)
        load("sp", invf, invf[:], k_invf, cch)
        load("sp", sgn, sgn[:], k_sgn, cch)
        memset("dve", onesb[:], 1.0, [onesb])
        memset("dve", zerosb[:], 0.0, [zerosb])

        BK = [TT(em, gs, f"bk{i}", [128, 512], F32, psum=True) for i in range(8)]

        class BfView:
            def __init__(self, bank):
                self.t = bank[:].bitcast(BF16).rearrange("p (c n) -> p c n", n=128)
                self.b = bank.b

            def __getitem__(self, k):
                return self.t[k]

        PS = Rot(BK[0:3])
        PT = Rot([BfView(BK[3]), BfView(BK[4])])

        def interleave(gens, width):
            pending = list(gens)
            active = [pending.pop(0) for _ in range(min(width, len(pending)))]
            while active:
                for g_ in list(active):
                    try:
                        next(g_)
                    except StopIteration:
                        active.remove(g_)
                        if pending:
                            active.append(pending.pop(0))
        PO = Rot(BK[5:7])
        psM = BK[7]

        def make_tables(s):
            with ExitStack() as st:
                posi = S(st, "posi", [128, T_], I32)
                ang = S(st, "ang", [128, T_])
                y = S(st, "ty", [128, T_])
                ki = S(st, "tki", [128, T_], I32)
                kf = S(st, "tkf", [128, T_])
                r = S(st, "tr", [128, T_])
                m = S(st, "tm", [128, T_])
                res = S(st, "tres", [128, T_])
                ch = em.new_chan(16, "tab")
                load("sp", posi, posi[:], pos_in[s:s + 1, :].to_broadcast([128, T_]), ch)
                cp("dve", ang[:], posi[:], [posi], [ang])
                for lay in range(2):
                    for kind in range(2):
                        ts("dve", y[:], ang[:], invf[:, lay:lay + 1], (math.pi / 2 if kind == 0 else 0.0),
                           ALU.mult, ALU.add, [ang, invf], [y])
                        ts("dve", r[:], y[:], 1.0 / (2 * math.pi), None, ALU.mult, None, [y], [r])
                        cp("dve", ki[:], r[:], [r], [ki])
                        cp("dve", kf[:], ki[:], [ki], [kf])
                        stt(r[:], kf[:], -6.28125, y[:], ALU.mult, ALU.add, [kf, y], [r])
                        stt(r[:], kf[:], -0.0019353071795864769, r[:], ALU.mult, ALU.add, [kf, r], [r])
                        ts("dve", m[:], r[:], math.pi, None, ALU.is_gt, None, [r], [m])
                        stt(r[:], m[:], -2 * math.pi, r[:], ALU.mult, ALU.add, [m, r], [r])
                        ts("dve", m[:], r[:], -math.pi, None, ALU.is_lt, None, [r], [m])
                        stt(r[:], m[:], 2 * math.pi, r[:], ALU.mult, ALU.add, [m, r], [r])
                        ts("dve", r[:], r[:], math.pi, -math.pi, ALU.min, ALU.max, [r], [r])
                        act(res[:], r[:], AF.Sin, [r], [res])
                        if kind == 1:
                            ts("dve", res[:], res[:], sgn[:, lay:lay + 1], None, ALU.mult, None, [res, sgn], [res])
                        store("sp", tab_scr[s, lay * 2 + kind], dbuf(f"tab{s}"), res, res[:], ch)
                em.barrier()

        class ApView:
            def __init__(self, ap, buf):
                self.t = ap
                self.b = buf

            def __getitem__(self, k):
                return self.t[k]

        def make_mod_all(modA):
            for l in range(n_layers):
                with ExitStack() as st:
                    craw = S(st, "craw", [128, 8, n_seq])
                    cs = S(st, "cs", [128, 8, n_seq], BF16)
                    abT = S(st, "abT", [128, 48])
                    ch = em.new_chan(16, "mod")
                    for s_ in range(n_seq):
                        load("sp", craw, craw[:, :, s_], c_in[s_].rearrange("(j p) -> p j", p=128), ch, slow=True)
                    ch2 = em.new_chan(16, "mod2")
                    load("sp", abT, abT[:], ada_b[l].rearrange("(j p) -> p j", p=128), ch2, slow=True)
                    act(cs[:], craw[:], AF.Silu, [craw], [cs])
                    wb = Rot([S(st, f"adaw{i}", [128, 8, 1024], BF16) for i in range(2)])
                    wch = [em.new_chan(16, "adaw") for _ in range(2)]
                    for blk in range(6):
                        w = wb.next()
                        for c in range(8):
                            load("pool", w, w[:, c, :], ada_w[l, c * 128:(c + 1) * 128, blk * 1024:(blk + 1) * 1024], wch[blk % 2])
                        for j in range(8):
                            col = blk * 8 + j
                            for c in range(8):
                                mm(psM[:, col * n_seq:(col + 1) * n_seq], w[:, c, j * 128:(j + 1) * 128], cs[:, c, :], c == 0, c == 7,
                                   [w, cs], [psM])
                    pv = psM[:, 0:48 * n_seq].rearrange("p (c s) -> p c s", s=n_seq)
                    for s_ in range(n_seq):
                        tt("dve", modA[:, l, s_, :], pv[:, :, s_], abT[:], ALU.add, [psM, abT], [modA])
                        for a in (8, 32):
                            ts("dve", modA[:, l, s_, a:a + 8], modA[:, l, s_, a:a + 8], 1.0, None, ALU.add, None, [modA], [modA])
                    em.barrier()

        def make_gate_bc(modT, col0, G, st):
            dg = S(st, "dg", [128, 128], BF16)
            for j in range(8):
                ts("dve", dg[:], identb[:], modT[:, col0 + j:col0 + j + 1], None, ALU.mult, None, [identb, modT], [dg])
                ps = PS.next()
                mm(ps[:, 0:128], onesb[:], dg[:], True, True, [onesb, dg], [ps])
                act(G[:, j * 128:(j + 1) * 128], ps[:, 0:128], AF.Identity, [ps], [G], bias=1.0, scale=1.0)

        def ln_to_hT(xsrc_ap, xsrc_buf, modT, sh_col, sc_col, hT, st, tag):
            W = LNW
            xt = [S(st, f"xt{tag}{i}", [128, 1024]) for i in range(W)]
            xch = [em.new_chan(16, "xt") for _ in range(W)]
            hn = [S(st, f"hn{tag}{i}", [128, 1024], BF16) for i in range(W)]
            stats = [S(st, f"st{tag}{i}", [128, 2, 6]) for i in range(W)]
            mv = [S(st, f"mv{tag}{i}", [128, 2]) for i in range(W)]
            rstd = [S(st, f"rstd{tag}{i}", [128, 1]) for i in range(W)]

            def gen(i):
                k = i % W
                load("sp", xt[k], xt[k][:], xsrc_ap[i * 128:(i + 1) * 128, :], xch[k], src_buf=dbuf(f"{xsrc_buf}_{i}"))
                yield
                yield from ln_tile_to_hT(xt[k], modT, sh_col, sc_col, hT, i, hn[k], stats[k], mv[k], rstd[k], psTs[k])
            psTs = [BfView(BK[k]) for k in range(W)]
            interleave([gen(i) for i in range(NT)], W)

        def ln_tile_to_hT(x_t, modT, sh_col, sc_col, hT, i, hn, stats, mv, rstd, psT):
            for c2 in range(2):
                em.op("dve", lambda e, c2=c2: e.bn_stats(out=stats[:, c2, :], in_=x_t[:, c2 * 512:(c2 + 1) * 512]),
                      [x_t], [stats])
            em.op("dve", lambda e: e.bn_aggr(out=mv[:], in_=stats[:].rearrange("p a b -> p (a b)")), [stats], [mv])
            yield
            act(rstd[:], mv[:, 1:2], AF.Sqrt, [mv], [rstd], bias=LN_EPS, scale=1.0)
            yield
            recip(rstd[:], rstd[:], [rstd], [rstd])
            ts("dve", hn[:], x_t[:], mv[:, 0:1], rstd[:], ALU.subtract, ALU.mult, [x_t, mv, rstd], [hn])
            yield
            for c in range(8):
                tr(psT[:, c, :], hn[:, c * 128:(c + 1) * 128], [hn, identb], [psT], identb[:])
            yield
            for c in range(8):
                act(hT[:, c, i * 128:(i + 1) * 128], psT[:, c, :], AF.Identity, [psT, modT], [hT],
                    bias=modT[:, sh_col + c:sh_col + c + 1], scale=modT[:, sc_col + c:sc_col + c + 1])
            yield

        def load_w_cols(st_w, name, l, col0, ncols, chs):
            w = S(st_w, name, [128, 8, ncols], BF16)
            for c in range(8):
                load("pool", w, w[:, c, :], w_in[l, c * 128:(c + 1) * 128, col0:col0 + ncols], chs)
            return w

        def make_rot(st_w, name, w, nblk, half, eng="dve"):
            wr = S(st_w, name, [128, 8, nblk * 2 * half], BF16)
            wv = w[:].rearrange("p c (b t h) -> p c b t h", t=2, h=half)
            rv = wr[:].rearrange("p c (b t h) -> p c b t h", t=2, h=half)
            for c in range(8):
                cp(eng, rv[:, c, :, 0, :], wv[:, c, :, 1, :], [w], [wr])
                cp(eng, rv[:, c, :, 1, :], wv[:, c, :, 0, :], [w], [wr])
            return wr

        def proj_fm(ps, w, c0, c1, hT, tg, nk=8):
            for c in range(nk):
                mm(ps[0:c1 - c0, :], w[:, c, c0:c1], hT[:, c, tg * 512:(tg + 1) * 512], c == 0, c == nk - 1,
                   [w, hT], [ps])

        def rope_evac(dst_ap, dst_t, psm, psr, cosT, sinT, rows, tg, scale, tmp1, tmp2, extra=None, dsts=None):
            r0, r1 = rows
            sl = slice(tg * 512, (tg + 1) * 512)
            stt(tmp1[r0:r1, :], psm[r0:r1, :], scale, cosT[r0:r1, sl], ALU.mult, ALU.mult, [psm, cosT], [tmp1])
            stt(tmp2[r0:r1, :], psr[r0:r1, :], scale, sinT[r0:r1, sl], ALU.mult, ALU.mult, [psr, sinT], [tmp2])
            if dsts is None:
                dsts = [(r0, r1, dst_ap)]
            if extra is not None:
                tt("dve", tmp1[r0:r1, :], tmp1[r0:r1, :], tmp2[r0:r1, :], ALU.add, [tmp1, tmp2], [tmp1])
                for (a, b, ap) in dsts:
                    tt("dve", ap, tmp1[a:b, :], extra[0][a:b, :], ALU.add, [tmp1, extra[1]], [dst_t])
            else:
                for (a, b, ap) in dsts:
                    tt("dve", ap, tmp1[a:b, :], tmp2[a:b, :], ALU.add, [tmp1, tmp2], [dst_t])

        def sumsq_max(src_ap, src_t, rows, statc, col, sq, scale=1.0):
            if isinstance(sq, Rot):
                sq = sq.next()
            tt("pool", sq[0:rows, :], src_ap, src_ap, ALU.mult, [src_t], [sq])
            ps = PS.next()
            mm(ps[:], onesb[0:rows, :], sq[0:rows, :], True, True, [onesb, sq], [ps])
            red(statc[:, col:col + 1], ps[:], ALU.max, [ps], [statc])

        def finish_negC(negC, statq, nq, statk, nk, kfac, st):
            qm = S(st, "qm" + negC.b.name, [128, 1])
            km = S(st, "km" + negC.b.name, [128, 1])
            red(qm[:], statq[:, 0:nq], ALU.max, [statq], [qm])
            red(km[:], statk[:, 0:nk], ALU.max, [statk], [km])
            tt("dve", qm[:], qm[:], km[:], ALU.mult, [qm, km], [qm])
            act(km[:], qm[:], AF.Sqrt, [qm], [km], scale=kfac * 1.1025)
            ts("dve", negC[:], km[:], -1.0, None, ALU.mult, None, [km], [negC])

        def mla_stage(s, l, hT):
            sc_mla = 1.0 / math.sqrt(96.0)
            with ExitStack() as st:
                ch = None
                cosM = S(st, "cosM", [96, T_])
                sinM = S(st, "sinM", [96, T_])
                load("sp", cosM, cosM[64:96, :], tab_scr[s, 2, 64:96, :], ch, src_buf=dbuf(f"tab{s}"))
                load("sp", sinM, sinM[64:96, :], tab_scr[s, 3, 64:96, :], ch, src_buf=dbuf(f"tab{s}"))
                QT = S(st, "QTm", [96, 4, T_], BF16)
                KT = S(st, "KTm", [96, 4, T_], BF16)
                V = S(st, "Vm", [128, NT, 4, 65], BF16)
                memset("pool", V[:], 1.0, [V])
                negC = S(st, "negCm", [128, 1])
                with ExitStack() as sw:
                    wcq = load_w_cols(sw, "wcq", l, O_CQ, 256, ch)
                    wckv = load_w_cols(sw, "wckv", l, O_CKV, 128, ch)
                    wkpe = S(sw, "wkpe", [128, 8, 96], BF16)
                    wkper = S(sw, "wkper", [128, 8, 96], BF16)
                    memset("pool", wkpe[:], 0.0, [wkpe])
                    memset("pool", wkper[:], 0.0, [wkper])
                    wsrc = w_in[l].rearrange("(c p) n -> p c n", p=128)
                    for c in range(8):
                        load("pool", wkpe, wkpe[:, c, 64:96], wsrc[:, c, O_KPE:O_KPE + 32], ch)
                        load("pool", wkper, wkper[:, c, 64:80], wsrc[:, c, O_KPE + 16:O_KPE + 32], ch)
                        load("pool", wkper, wkper[:, c, 80:96], wsrc[:, c, O_KPE:O_KPE + 16], ch)
                    qn = S(sw, "qn", [128, 2])
                    kvn = S(sw, "kvn", [128, 1])
                    load("sp", qn, qn[:], mla_qn[l].rearrange("(c p) -> p c", p=128), ch, slow=True)
                    load("sp", kvn, kvn[:], mla_kvn[l].rearrange("(c p) -> p c", p=128), ch, slow=True)
                    wuq = S(sw, "wuq", [128, 2, 384], BF16)
                    wuqr = S(sw, "wuqr", [128, 2, 384], BF16)
                    load("pool", wuq, wuq[:], mla_wuq[l].rearrange("(c p) n -> p c n", p=128), ch)
                    for c in range(2):
                        ts("dve", wuq[:, c, :], wuq[:, c, :], qn[:, c:c + 1], None, ALU.mult, None, [wuq, qn], [wuq])
                    cp("dve", wuqr[:], wuq[:], [wuq], [wuqr])
                    wv4 = wuq[:].rearrange("p c (h d) -> p c h d", d=96)
                    rv4 = wuqr[:].rearrange("p c (h d) -> p c h d", d=96)
                    for c in range(2):
                        cp("dve", rv4[:, c, :, 64:80], wv4[:, c, :, 80:96], [wuq], [wuqr])
                        cp("dve", rv4[:, c, :, 80:96], wv4[:, c, :, 64:80], [wuq], [wuqr])
                    wukv = S(sw, "wukv", [128, 512], BF16)
                    load("pool", wukv, wukv[:], mla_wukv[l], ch)
                    ts("dve", wukv[:], wukv[:], kvn[:, 0:1], None, ALU.mult, None, [wukv, kvn], [wukv])
                    cqf = S(sw, "cqf", [128, 2, 512])
                    sq = S(sw, "sqm", [128, 2, 512], BF16)
                    Rq = S(sw, "Rq", [128, 512])
                    cqn = S(sw, "cqn", [128, 2, 512], BF16)
                    ckvf = S(sw, "ckvf", [128, 512])
                    ckvn = S(sw, "ckvn", [128, 512], BF16)
                    t1 = S(sw, "t1m", [128, 512])
                    t2 = S(sw, "t2m", [128, 512])
                    kpe = S(sw, "kpe", [96, 512], BF16)
                    for tg in range(4):
                        for hc in range(2):
                            ps = PS.next()
                            proj_fm(ps, wcq, hc * 128, (hc + 1) * 128, hT, tg)
                            cp("act", cqf[:, hc, :], ps[:], [ps], [cqf])
                            act(sq[:, hc, :], ps[:], AF.Square, [ps], [sq])
                        ps = PS.next()
                        for hc in range(2):
                            mm(ps[:], onesb[:], sq[:, hc, :], hc == 0, hc == 1, [onesb, sq], [ps])
                        act(Rq[:], ps[:], AF.Sqrt, [ps], [Rq], bias=RMS_EPS, scale=1.0 / 256.0)
                        recip(Rq[:], Rq[:], [Rq], [Rq])
                        for hc in range(2):
                            tt("dve", cqn[:, hc, :], cqf[:, hc, :], Rq[:], ALU.mult, [cqf, Rq], [cqn])
                        ps = PS.next()
                        proj_fm(ps, wckv, 0, 128, hT, tg)
                        cp("act", ckvf[:], ps[:], [ps], [ckvf])
                        act(sq[:, 0, :], ps[:], AF.Square, [ps], [sq])
                        ps = PS.next()
                        mm(ps[:], onesb[:], sq[:, 0, :], True, True, [onesb, sq], [ps])
                        act(Rq[:], ps[:], AF.Sqrt, [ps], [Rq], bias=RMS_EPS, scale=1.0 / 128.0)
                        recip(Rq[:], Rq[:], [Rq], [Rq])
                        tt("dve", ckvn[:], ckvf[:], Rq[:], ALU.mult, [ckvf, Rq], [ckvn])
                        for h in range(4):
                            psq = PS.next()
                            psr = PS.next()
                            for c in range(2):
                                mm(psq[0:96, :], wuq[:, c, h * 96:(h + 1) * 96], cqn[:, c, :], c == 0, c == 1,
                                   [wuq, cqn], [psq])
                            for c in range(2):
                                mm(psr[0:96, :], wuqr[:, c, h * 96:(h + 1) * 96], cqn[:, c, :], c == 0, c == 1,
                                   [wuqr, cqn], [psr])
                            act(QT[0:64, h, tg * 512:(tg + 1) * 512], psq[0:64, :], AF.Copy, [psq], [QT], scale=sc_mla)
                            rope_evac(QT[64:96, h, tg * 512:(tg + 1) * 512], QT, psq, psr, cosM, sinM, (64, 96), tg,
                                      sc_mla, t1, t2)
                        for h in range(4):
                            ps = PS.next()
                            mm(ps[0:64, :], wukv[:, h * 128:h * 128 + 64], ckvn[:], True, True, [wukv, ckvn], [ps])
                            cp("act", KT[0:64, h, tg * 512:(tg + 1) * 512], ps[0:64, :], [ps], [KT])
                        psk = PS.next()
                        psr = PS.next()
                        proj_fm(psk, wkpe, 0, 96, hT, tg)
                        proj_fm(psr, wkper, 0, 96, hT, tg)
                        rope_evac(kpe[64:96, :], kpe, psk, psr, cosM, sinM, (64, 96), tg, 1.0, t1, t2)
                        for h in range(4):
                            cp("act", KT[64:96, h, tg * 512:(tg + 1) * 512], kpe[64:96, :], [kpe], [KT])
                        wv = wukv[:].rearrange("p (h d) -> p h d", d=128)
                        for tt_ in range(4):
                            ps = PS.next()
                            mm(ps[:, 0:256].rearrange("p (h d) -> p h d", d=64), ckvn[:, tt_ * 128:(tt_ + 1) * 128],
                               wv[:, :, 64:128], True, True, [ckvn, wukv], [ps])
                            cp("act", V[:, tg * 4 + tt_, :, 0:64], ps[:, 0:256].rearrange("p (h d) -> p h d", d=64),
                               [ps], [V])
                    statq = S(sw, "statqm", [128, 16])
                    statk = S(sw, "statkm", [128, 16])
                    sqn = Rot([S(sw, f"sqnm{i}", [128, 512], BF16) for i in range(3)])
                    for h in range(4):
                        for tg in range(4):
                            sumsq_max(QT[0:96, h, tg * 512:(tg + 1) * 512], QT, 96, statq, h * 4 + tg, sqn)
                            sumsq_max(KT[0:96, h, tg * 512:(tg + 1) * 512], KT, 96, statk, h * 4 + tg, sqn)
                    finish_negC(negC, statq, 16, statk, 16, 1.0, sw)
                    em.barrier()
                pT = Rot([S(st, f"pTm{i}", [128, 512], BF16) for i in range(3)])
                ot = Rot([S(st, f"otm{i}", [128, 256], BF16) for i in range(2)])
                rden = S(st, "rdenm", [128, 1])
                och = [em.new_chan(16, "om") for _ in range(2)]
                for i in range(NT):
                    o_t = ot.next()
                    for h in range(4):
                        po = PO.next()
                        chunks = [list(range(j0, min(j0 + 4, i + 1))) for j0 in range(0, i + 1, 4)]
                        pend = None
                        for ci in range(len(chunks) + 1):
                            cur = None
                            if ci < len(chunks):
                                js = chunks[ci]
                                n = len(js) * 128
                                ps = PS.next()
                                mm(ps[:, 0:n], identb[:], zerosb[:, 0:n], True, False, [identb, zerosb], [ps])
                                for jj, j in enumerate(js):
                                    mm(ps[:, jj * 128:(jj + 1) * 128], KT[0:96, h, j * 128:(j + 1) * 128],
                                       QT[0:96, h, i * 128:(i + 1) * 128], False, (jj == len(js) - 1 and j != i), [KT, QT], [ps])
                                    if j == i:
                                        mm(ps[:, jj * 128:(jj + 1) * 128], identb[:], mdiag4[:, 0:128], False, True,
                                           [identb, mdiag4], [ps])
                                p_t = pT.next()
                                act(p_t[:, 0:n], ps[:, 0:n], AF.Exp, [ps, negC], [p_t], bias=negC[:, 0:1], scale=1.0)
                                cur = (js, p_t)
                            if pend is not None:
                                jsp, p_p = pend
                                for jj, j in enumerate(jsp):
                                    mm(po[:, 0:65], p_p[:, jj * 128:(jj + 1) * 128], V[:, j, h, :], j == 0, j == i,
                                       [p_p, V], [po])
                            pend = cur
                        recip(rden[:], po[:, 64:65], [po], [rden])
                        ts("dve", o_t[:, h * 64:(h + 1) * 64], po[:, 0:64], rden[:, 0:1], None, ALU.mult, None,
                           [po, rden], [o_t])
                    store("sp", o_scr[s, i * 128:(i + 1) * 128, 0:256], dbuf(f"o{s}_{i}"), o_t, o_t[:], och[i % 2])
                em.barrier()

        def swa_stage(s, l, hT):
            with ExitStack() as st:
                ch = None
                cosH = S(st, "cosH", [128, T_])
                sinH = S(st, "sinH", [128, T_])
                load("sp", cosH, cosH[:], tab_scr[s, 0], ch, src_buf=dbuf(f"tab{s}"))
                load("sp", sinH, sinH[:], tab_scr[s, 1], ch, src_buf=dbuf(f"tab{s}"))
                QT = S(st, "QTs", [128, 2, T_], BF16)
                KT = S(st, "KTs", [128, 2, 2, T_], BF16)
                memset("pool", KT[:], 0.0, [KT])
                V = S(st, "Vs", [128, NT, 2, 65], BF16)
                memset("pool", V[:], 1.0, [V])
                negC = S(st, "negCs", [128, 1])
                sink = S(st, "sink", [128, 4])
                sinke = S(st, "sinke", [128, 4])
                load("sp", sink, sink[:], swa_sinks[l:l + 1, :].to_broadcast([128, 4]), ch)
                with ExitStack() as sw:
                    wq = load_w_cols(sw, "wsq", l, O_SQ, 256, ch)
                    wqr = make_rot(sw, "wsqr", wq, 4, 32)
                    wk1 = load_w_cols(sw, "wsk", l, O_SK, 128, ch)
                    wk = S(sw, "wskd", [128, 8, 2, 128], BF16)
                    for g in range(2):
                        for hf in range(2):
                            cp("dve", wk[:, :, g, hf * 64:(hf + 1) * 64], wk1[:, :, g * 64:(g + 1) * 64], [wk1], [wk])
                    wkf = S(sw, "wskdf", [128, 8, 256], BF16)
                    cp("dve", wkf[:], wk[:].rearrange("p c g d -> p c (g d)"), [wk], [wkf])
                    wkr = make_rot(sw, "wskr", wkf, 4, 32)
                    wv = load_w_cols(sw, "wsv", l, O_SV, 128, ch)
                    t1 = S(sw, "t1s", [128, 512])
                    t2 = S(sw, "t2s", [128, 512])
                    for tg in range(4):
                        for p in range(2):
                            psm = PS.next()
                            psr = PS.next()
                            proj_fm(psm, wq, p * 128, (p + 1) * 128, hT, tg)
                            proj_fm(psr, wqr, p * 128, (p + 1) * 128, hT, tg)
                            rope_evac(QT[:, p, tg * 512:(tg + 1) * 512], QT, psm, psr, cosH, sinH, (0, 128), tg,
                                      0.125, t1, t2)
                        for g in range(2):
                            psm = PS.next()
                            psr = PS.next()
                            proj_fm(psm, wkf, g * 128, (g + 1) * 128, hT, tg)
                            proj_fm(psr, wkr, g * 128, (g + 1) * 128, hT, tg)
                            rope_evac(None, KT, psm, psr, cosH, sinH, (0, 128), tg, 1.0, t1, t2,
                                      dsts=[(0, 64, KT[0:64, g, 0, tg * 512:(tg + 1) * 512]),
                                            (64, 128, KT[64:128, g, 1, tg * 512:(tg + 1) * 512])])
                    for i in range(NT):
                        ps = PS.next()
                        for c in range(8):
                            mm(ps[:, 0:128], hT[:, c, i * 128:(i + 1) * 128], wv[:, c, :], c == 0, c == 7, [hT, wv], [ps])
                        cp("act", V[:, i, :, 0:64], ps[:, 0:128].rearrange("p (g d) -> p g d", d=64), [ps], [V])
                    statq = S(sw, "statqs", [128, 8])
                    statk = S(sw, "statks", [128, 8])
                    sqn = Rot([S(sw, f"sqns{i}", [128, 512], BF16) for i in range(3)])
                    for p in range(2):
                        for tg in range(4):
                            sumsq_max(QT[:, p, tg * 512:(tg + 1) * 512], QT, 128, statq, p * 4 + tg, sqn)
                            sumsq_max(KT[:, p, 0, tg * 512:(tg + 1) * 512], KT, 128, statk, p * 4 + tg, sqn)
                    finish_negC(negC, statq, 8, statk, 8, 1.0, sw)
                    act(sinke[:], sink[:], AF.Exp, [sink, negC], [sinke], bias=negC[:, 0:1], scale=1.0)
                    em.barrier()
                pT = Rot([S(st, f"pTs{i}", [128, 512], BF16) for i in range(3)])
                ot = Rot([S(st, f"ots{i}", [128, 256], BF16) for i in range(2)])
                den = S(st, "dens", [128, 1])
                och = [em.new_chan(16, "os") for _ in range(2)]
                for i in range(NT):
                    o_t = ot.next()
                    for g in range(2):
                        js = [i - 1, i] if i > 0 else [i]
                        n = len(js) * 256
                        ps = PS.next()
                        msk = mwin4 if i > 0 else mdiag4
                        mm(ps[:, 0:n], identb[:], msk[:, 0:n], True, False, [identb, msk], [ps])
                        for jj, j in enumerate(js):
                            for hf in range(2):
                                col = jj * 256 + hf * 128
                                mm(ps[:, col:col + 128], KT[:, g, hf, j * 128:(j + 1) * 128],
                                   QT[:, g, i * 128:(i + 1) * 128], False,
                                   (jj == len(js) - 1 and hf == 1), [KT, QT], [ps])
                        p_t = pT.next()
                        act(p_t[:, 0:n], ps[:, 0:n], AF.Exp, [ps, negC], [p_t], bias=negC[:, 0:1], scale=1.0)
                        for hf in range(2):
                            h = 2 * g + hf
                            po = PO.next()
                            for jj, j in enumerate(js):
                                col = jj * 256 + hf * 128
                                mm(po[:, 0:65], p_t[:, col:col + 128], V[:, j, g, :], jj == 0, jj == len(js) - 1,
                                   [p_t, V], [po])
                            tt("dve", den[:], po[:, 64:65], sinke[:, h:h + 1], ALU.add, [po, sinke], [den])
                            recip(den[:], den[:], [den], [den])
                            ts("dve", o_t[:, h * 64:(h + 1) * 64], po[:, 0:64], den[:, 0:1], None, ALU.mult, None,
                               [po, den], [o_t])
                    store("sp", o_scr[s, i * 128:(i + 1) * 128, 256:512], dbuf(f"o{s}_{i}"), o_t, o_t[:], och[i % 2])
                em.barrier()

        def nsa_stage(s, l, hT):
            with ExitStack() as st:
                ch = None
                cosH = S(st, "cosHn", [128, T_])
                sinH = S(st, "sinHn", [128, T_])
                load("sp", cosH, cosH[:], tab_scr[s, 0], ch, src_buf=dbuf(f"tab{s}"))
                load("sp", sinH, sinH[:], tab_scr[s, 1], ch, src_buf=dbuf(f"tab{s}"))
                cmpmask = S(st, "cmpmask", [128, NT, 256], BF16)
                keepM = S(st, "keepM", [128, NT, 64], BF16)
                addM = S(st, "addM", [128, NT, 64], BF16)
                expandE = S(st, "expandE", [128, T_], BF16)
                QT = S(st, "QTn", [128, 4, T_], BF16)
                KwT = S(st, "KwT", [128, 2, 2, T_], BF16)
                KsT = S(st, "KsT", [128, 2, 2, T_], BF16)
                Vs = S(st, "Vns", [128, NT, 2, 65], BF16)
                Vw = S(st, "Vnw", [128, NT, 2, 65], BF16)
                gates = S(st, "gates", [128, NT, 24])
                KcmpT = S(st, "KcmpT", [128, 2, 2, 64], BF16)
                Vcmp = S(st, "Vcmp", [128, 2, 64], BF16)
                negCw = S(st, "negCw", [128, 1])
                negCs = S(st, "negCsl", [128, 1])
                with ExitStack() as sw:
                    KcT = S(sw, "KcT", [128, T_], BF16)
                    VcT = S(sw, "VcT", [128, T_], BF16)
                    t1 = S(sw, "t1n", [128, 512])
                    t2 = S(sw, "t2n", [128, 512])
                    cpT = S(sw, "cpT", [128, 32])
                    cpt = S(sw, "cpt", [128, 512])
                    for g in range(2):
                        load("sp", cpT, cpT[g * 64:(g + 1) * 64, :], cmp_pos[l].rearrange("i d -> d i"), ch, slow=True)
                    for b in range(16):
                        cp("dve", cpt[:, b * 32:(b + 1) * 32], cpT[:], [cpT], [cpt])
                    phi1s = [S(sw, "phi1k", [128, 32, 256], BF16), S(sw, "phi1v", [128, 32, 256], BF16)]
                    phich = [em.new_chan(16, "phik"), em.new_chan(16, "phiv")]
                    raw = {nm: S(sw, "wraw" + nm, [128, 8, nc_], BF16) for nm, nc_ in
                           (("kw", 128), ("ks", 128), ("kc", 128), ("vc", 128), ("tm", 280))}

                    def load_raw():
                        wsrc_ = w_in[l].rearrange("(c p) n -> p c n", p=128)
                        for nm, off in (("kw", O_NKW), ("ks", O_NKS), ("kc", O_NKC), ("vc", O_NVC)):
                            for c in range(8):
                                load("pool", raw[nm], raw[nm][:, c, :], wsrc_[:, c, off:off + 128], None)
                        for c in range(8):
                            load("pool", raw["tm"], raw["tm"][:, c, 0:128], wsrc_[:, c, O_NVS:O_NVS + 128], None)
                            load("pool", raw["tm"], raw["tm"][:, c, 128:256], wsrc_[:, c, O_NVW:O_NVW + 128], None)
                            load("pool", raw["tm"], raw["tm"][:, c, 256:280], wsrc_[:, c, O_NG:O_NG + 24], None)
                    with ExitStack() as sp_:
                        wq = load_w_cols(sp_, "wnq", l, O_NQ, 512, ch)
                        load_raw()
                        wqr = make_rot(sp_, "wnqr", wq, 8, 32)
                        def late_init():
                            load("pool", cmpmask, cmpmask[:], k_cmpmask.rearrange("p (a b) -> p a b", b=256), ch)
                            load("pool", keepM, keepM[:], k_keep.rearrange("p (a b) -> p a b", b=64), ch)
                            load("pool", addM, addM[:], k_add.rearrange("p (a b) -> p a b", b=64), ch)
                            load("pool", expandE, expandE[:], k_expand, ch)
                            memset("pool", KwT[:], 0.0, [KwT])
                            memset("pool", KsT[:], 0.0, [KsT])
                            memset("pool", Vs[:], 1.0, [Vs])
                            memset("pool", Vw[:], 1.0, [Vw])
                            memset("pool", KcmpT[:], 0.0, [KcmpT])
                            memset("pool", Vcmp[:], 0.0, [Vcmp])
                            for which, phi in ((0, phi_k1), (1, phi_v1)):
                                for g in range(2):
                                    for i4 in range(8):
                                        load("pool", phi1s[which], phi1s[which][g * 64:(g + 1) * 64, i4 * 4:(i4 + 1) * 4, :],
                                             phi[l, i4 * 256:(i4 + 1) * 256, :].rearrange("(i d) h -> d i h", d=64), phich[which])
                        for tg in range(4):
                            for p in range(4):
                                psm = PS.next()
                                psr = PS.next()
                                proj_fm(psm, wq, p * 128, (p + 1) * 128, hT, tg)
                                proj_fm(psr, wqr, p * 128, (p + 1) * 128, hT, tg)
                                rope_evac(QT[:, p, tg * 512:(tg + 1) * 512], QT, psm, psr, cosH, sinH, (0, 128), tg,
                                          0.125, t1, t2)
                        late_init()
                        em.barrier()
                    for (off, dst, nm) in ((O_NKW, KwT, "kw"), (O_NKS, KsT, "ks")):
                        with ExitStack() as sp_:
                            wk1 = raw[nm]
                            wkf = S(sp_, "wnf" + nm, [128, 8, 256], BF16)
                            wkf4 = wkf[:].rearrange("p c (g f d) -> p c g f d", g=2, f=2)
                            for g in range(2):
                                for hf in range(2):
                                    cp("dve", wkf4[:, :, g, hf, :], wk1[:, :, g * 64:(g + 1) * 64], [wk1], [wkf])
                            wkr = make_rot(sp_, "wnr" + nm, wkf, 4, 32)
                            for tg in range(4):
                                for g in range(2):
                                    psm = PS.next()
                                    psr = PS.next()
                                    proj_fm(psm, wkf, g * 128, (g + 1) * 128, hT, tg)
                                    proj_fm(psr, wkr, g * 128, (g + 1) * 128, hT, tg)
                                    rope_evac(None, dst, psm, psr, cosH, sinH, (0, 128), tg, 1.0, t1, t2,
                                              dsts=[(0, 64, dst[0:64, g, 0, tg * 512:(tg + 1) * 512]),
                                                    (64, 128, dst[64:128, g, 1, tg * 512:(tg + 1) * 512])])
                            em.barrier()
                    with ExitStack() as sp_:
                        wkc = raw["kc"]
                        wkcr = make_rot(sp_, "wnkcr", wkc, 2, 32)
                        wvc = raw["vc"]
                        wtm = raw["tm"]
                        for tg in range(4):
                            psm = PS.next()
                            psr = PS.next()
                            proj_fm(psm, wkc, 0, 128, hT, tg)
                            proj_fm(psr, wkcr, 0, 128, hT, tg)
                            rope_evac(KcT[:, tg * 512:(tg + 1) * 512], KcT, psm, psr, cosH, sinH, (0, 128), tg, 1.0,
                                      t1, t2, extra=(cpt, cpt))
                            psm = PS.next()
                            proj_fm(psm, wvc, 0, 128, hT, tg)
                            tt("dve", VcT[:, tg * 512:(tg + 1) * 512], psm[:], cpt[:], ALU.add, [psm, cpt], [VcT])
                        for i in range(NT):
                            ps = PS.next()
                            for c in range(8):
                                mm(ps[:, 0:280], hT[:, c, i * 128:(i + 1) * 128], wtm[:, c, :], c == 0, c == 7,
                                   [hT, wtm], [ps])
                            cp("act", Vs[:, i, :, 0:64], ps[:, 0:128].rearrange("p (g d) -> p g d", d=64), [ps], [Vs])
                            cp("act", Vw[:, i, :, 0:64], ps[:, 128:256].rearrange("p (g d) -> p g d", d=64), [ps], [Vw])
                            act(gates[:, i, :], ps[:, 256:280], AF.Sigmoid, [ps], [gates])
                        em.barrier()
                    with ExitStack() as sp_:
                        phi2k = S(sp_, "phi2k", [128, 2, 128], BF16)
                        phi2v = S(sp_, "phi2v", [128, 2, 64], BF16)
                        hid = S(sp_, "hid", [128, 2, 128], BF16)
                        gx = S(sp_, "gx", [128, 128])
                        gu = S(sp_, "gu", [128, 128])
                        for hf in range(2):
                            load("pool", phi2k, phi2k[:, :, hf * 64:(hf + 1) * 64],
                                 phi_k2[l].rearrange("(c p) d -> p c d", p=128), ch)
                        load("pool", phi2v, phi2v[:], phi_v2[l].rearrange("(c p) d -> p c d", p=128), ch)
                        for which, src, phi in ((0, KcT, phi_k1), (1, VcT, phi_v1)):
                            phi1 = phi1s[which]
                            for hc in range(2):
                                for g in range(2):
                                    ps = PS.next()
                                    for i in range(32):
                                        mm(ps[:, 0:64], phi1[g * 64:(g + 1) * 64, i, hc * 128:(hc + 1) * 128],
                                           src[g * 64:(g + 1) * 64, i::32], i == 0, i == 31, [phi1, src], [ps])
                                    cp("act", gx[:, g * 64:(g + 1) * 64], ps[:, 0:64], [ps], [gx])
                                tt("dve", gu[:], gx[:], gx[:], ALU.mult, [gx], [gu])
                                ts("dve", gu[:], gu[:], 0.044715, 1.0, ALU.mult, ALU.add, [gu], [gu])
                                tt("dve", gu[:], gu[:], gx[:], ALU.mult, [gu, gx], [gu])
                                act(gu[:], gu[:], AF.Sigmoid, [gu], [gu], scale=1.5957691216057308)
                                tt("dve", hid[:, hc, :], gu[:], gx[:], ALU.mult, [gu, gx], [hid])
                            if which == 0:
                                ps = PS.next()
                                for hc in range(2):
                                    mm(ps[:, 0:128], phi2k[:, hc, :], hid[:, hc, :], hc == 0, hc == 1, [phi2k, hid], [ps])
                                cp("act", KcmpT[0:64, :, 0, :], ps[0:64, 0:128].rearrange("p (g c) -> p g c", c=64), [ps], [KcmpT])
                                cp("act", KcmpT[64:128, :, 1, :], ps[64:128, 0:128].rearrange("p (g c) -> p g c", c=64), [ps], [KcmpT])
                            else:
                                for g in range(2):
                                    ps = PS.next()
                                    for hc in range(2):
                                        mm(ps[0:64, 0:64], hid[:, hc, g * 64:(g + 1) * 64], phi2v[:, hc, :], hc == 0,
                                           hc == 1, [hid, phi2v], [ps])
                                    cp("act", Vcmp[0:64, g, :], ps[0:64, 0:64], [ps], [Vcmp])
                        em.barrier()
                    statq = S(sw, "statqn", [128, 16])
                    statkw = S(sw, "statkw", [128, 8])
                    statks = S(sw, "statks2", [128, 8])
                    sqn = Rot([S(sw, f"sqnn{i}", [128, 512], BF16) for i in range(3)])
                    for p in range(4):
                        for tg in range(4):
                            sumsq_max(QT[:, p, tg * 512:(tg + 1) * 512], QT, 128, statq, p * 4 + tg, sqn)
                    for g in range(2):
                        for tg in range(4):
                            sumsq_max(KwT[:, g, 0, tg * 512:(tg + 1) * 512], KwT, 128, statkw, g * 4 + tg, sqn)
                            sumsq_max(KsT[:, g, 0, tg * 512:(tg + 1) * 512], KsT, 128, statks, g * 4 + tg, sqn)
                    finish_negC(negCw, statq, 16, statkw, 8, 1.0, sw)
                    finish_negC(negCs, statq, 16, statks, 8, 1.0, sw)
                    em.barrier()
                selb_all = S(st, "selb_all", [128, NT, 2, 64], BF16)
                ocmp_all = S(st, "ocmp_all", [128, NT, 512], BF16)
                with ExitStack() as sA:
                    W = 4
                    mx_ = [S(sA, f"mx{k}", [128, 4]) for k in range(W)]
                    den_ = [S(sA, f"denn{k}", [128, 4]) for k in range(W)]
                    ee_ = [S(sA, f"ee{k}", [128, 4, 64]) for k in range(W)]
                    pp_ = [S(sA, f"ppn{k}", [128, 4, 64]) for k in range(W)]
                    pb_ = [S(sA, f"pbn{k}", [128, 4, 64], BF16) for k in range(W)]
                    pcT_ = [S(sA, f"pcT{k}", [128, 4, 128], BF16) for k in range(W)]
                    imp_ = [S(sA, f"imp{k}", [128, 64]) for k in range(W)]
                    max8_ = [S(sA, f"max8{k}", [128, 8]) for k in range(W)]
                    for k in range(W):
                        memset("pool", pcT_[k][:], 0.0, [pcT_[k]])

                    def genA(idx):
                        i, g = idx // 2, idx % 2
                        k = idx % W
                        mx, den, ee, pp, pb, pcT, imp, max8 = mx_[k], den_[k], ee_[k], pp_[k], pb_[k], pcT_[k], imp_[k], max8_[k]
                        psc = BK[2 * k]
                        mm(psc[:, 0:256], identb[:], cmpmask[:, i, :], True, False, [identb, cmpmask], [psc])
                        for sl in range(4):
                            hf, pl = sl // 2, sl % 2
                            mm(psc[:, sl * 64:(sl + 1) * 64], QT[:, 2 * g + pl, i * 128:(i + 1) * 128],
                               KcmpT[:, g, hf, :], False, sl == 3, [QT, KcmpT], [psc])
                        yield
                        red(mx[:], psc[:, 0:256].rearrange("p (s c) -> p s c", c=64), ALU.max, [psc], [mx])
                        ts("dve", mx[:], mx[:], -10000.0, -1.0, ALU.max, ALU.mult, [mx], [mx])
                        yield
                        for sl in range(4):
                            act(ee[:, sl, :], psc[:, sl * 64:(sl + 1) * 64], AF.Exp, [psc, mx], [ee, den],
                                bias=mx[:, sl:sl + 1], scale=1.0, accum=den[:, sl:sl + 1])
                        yield
                        ts("dve", den[:], den[:], 1e-30, None, ALU.max, None, [den], [den])
                        recip(den[:], den[:], [den], [den])
                        tt("dve", pp[:], ee[:], den[:].unsqueeze(2).to_broadcast([128, 4, 64]), ALU.mult, [ee, den], [pp])
                        cp("pool", pb[:], pp[:], [pp], [pb])
                        red(imp[:], pp[:].rearrange("p s c -> p c s"), ALU.add, [pp], [imp])
                        yield
                        psT = BfView(BK[2 * k + 1])
                        for sl in range(4):
                            tr(psT[0:64, sl, :], pb[:, sl, :], [pb, identb], [psT], identb[:])
                        yield
                        cp("act", pcT[0:64, :, :], psT[0:64, 0:4, :], [psT], [pcT])
                        tt("dve", imp[:], imp[:], keepM[:, i, :], ALU.mult, [imp, keepM], [imp])
                        tt("dve", imp[:], imp[:], addM[:, i, :], ALU.add, [imp, addM], [imp])
                        em.op("dve", lambda e: e.max(out=max8[:], in_=imp[:]), [imp], [max8])
                        yield
                        po = BK[2 * k + 1]
                        for sl in range(4):
                            mm(po[:, sl * 64:(sl + 1) * 64], pcT[:, sl, :], Vcmp[:, g, :], True, True, [pcT, Vcmp], [po])
                        ts("dve", imp[:], imp[:], max8[:, 7:8], None, ALU.is_ge, None, [imp, max8], [imp])
                        ts("dve", selb_all[:, i, g, :], imp[:], -NEG, NEG, ALU.mult, ALU.add, [imp], [selb_all])
                        yield
                        for sl in range(4):
                            hf, pl = sl // 2, sl % 2
                            h = 4 * g + 2 * pl + hf
                            act(ocmp_all[:, i, h * 64:(h + 1) * 64], po[:, sl * 64:(sl + 1) * 64], AF.Identity, [po, gates], [ocmp_all],
                                scale=gates[:, i, 3 * h:3 * h + 1])
                        yield
                    interleave([genA(idx) for idx in range(NT * 2)], W)
                    em.barrier()
                pT = Rot([S(st, f"pTn{i}", [128, 512], BF16) for i in range(4)])
                ot = Rot([S(st, f"otn{i}", [128, 512], BF16) for i in range(2)])
                och = [em.new_chan(16, "on") for _ in range(2)]
                nsel_r = Rot([S(st, f"nselT{i}", [128, 4, 128], BF16) for i in range(2)])
                for nt_ in nsel_r.items:
                    memset("pool", nt_[:], 0.0, [nt_])
                gr_r = Rot([S(st, f"grn{i}", [128, 1]) for i in range(4)])
                for i in range(NT):
                    o_t = ot.next()
                    for g in range(2):
                        nselT = nsel_r.next()
                        psT = PT.next()
                        tr(psT[0:64, 0, :], selb_all[:, i, g, :], [selb_all, identb], [psT], identb[:])
                        cp("act", nselT[0:64, :, :], psT[0:64, 0:1, :].to_broadcast([64, 4, 128]), [psT], [nselT])
                        po = PO.next()
                        mm(po[:, 0:260], zerosb[:, 0:128], zerosb[:, 0:260], True, False, [zerosb], [po])
                        pend = None
                        for j in range(i + 2):
                            cur = None
                            if j <= i:
                                ps = PS.next()
                                mm(ps[:], expandE[:, j * 128:(j + 1) * 128], nselT[:].rearrange("p s q -> p (s q)"), True,
                                   False, [expandE, nselT], [ps])
                                if j == i:
                                    mm(ps[:], identb[:], mdiag4[:], False, False, [identb, mdiag4], [ps])
                                for hf in range(2):
                                    mm(ps[:, hf * 256:(hf + 1) * 256].rearrange("p (a q) -> p a q", q=128),
                                       KsT[:, g, hf, j * 128:(j + 1) * 128],
                                       QT[:, 2 * g:2 * g + 2, i * 128:(i + 1) * 128], False, hf == 1,
                                       [KsT, QT], [ps])
                                p_t = pT.next()
                                act(p_t[:], ps[:], AF.Exp, [ps, negCs], [p_t], bias=negCs[:, 0:1], scale=1.0)
                                cur = (j, p_t)
                            if pend is not None:
                                jp, p_p = pend
                                for sl in range(4):
                                    mm(po[:, sl * 65:(sl + 1) * 65], p_p[:, sl * 128:(sl + 1) * 128], Vs[:, jp, g, :], False,
                                       (jp == i and sl == 3), [p_p, Vs], [po])
                            pend = cur
                        for sl in range(4):
                            hf, pl = sl // 2, sl % 2
                            h = 4 * g + 2 * pl + hf
                            gr = gr_r.next()
                            recip(gr[:], po[:, sl * 65 + 64:sl * 65 + 65], [po], [gr])
                            tt("dve", gr[:], gr[:], gates[:, i, 3 * h + 1:3 * h + 2], ALU.mult, [gr, gates], [gr])
                            stt(o_t[:, h * 64:(h + 1) * 64], po[:, sl * 65:sl * 65 + 64], gr[:, 0:1],
                                ocmp_all[:, i, h * 64:(h + 1) * 64], ALU.mult, ALU.add, [po, gr, ocmp_all], [o_t])
                        js = [i - 1, i] if i > 0 else [i]
                        pts = []
                        for j in js:
                            ps = PS.next()
                            msk = mdiag4 if j == i else mprev4
                            mm(ps[:], identb[:], msk[:], True, False, [identb, msk], [ps])
                            for hf in range(2):
                                mm(ps[:, hf * 256:(hf + 1) * 256].rearrange("p (a q) -> p a q", q=128),
                                   KwT[:, g, hf, j * 128:(j + 1) * 128],
                                   QT[:, 2 * g:2 * g + 2, i * 128:(i + 1) * 128], False, hf == 1,
                                   [KwT, QT], [ps])
                            p_t = pT.next()
                            act(p_t[:], ps[:], AF.Exp, [ps, negCw], [p_t], bias=negCw[:, 0:1], scale=1.0)
                            pts.append(p_t)
                        po = PO.next()
                        for sl in range(4):
                            for jj, j in enumerate(js):
                                mm(po[:, sl * 65:(sl + 1) * 65], pts[jj][:, sl * 128:(sl + 1) * 128], Vw[:, j, g, :],
                                   jj == 0, jj == len(js) - 1, [pts[jj], Vw], [po])
                        for sl in range(4):
                            hf, pl = sl // 2, sl % 2
                            h = 4 * g + 2 * pl + hf
                            gr = gr_r.next()
                            recip(gr[:], po[:, sl * 65 + 64:sl * 65 + 65], [po], [gr])
                            tt("dve", gr[:], gr[:], gates[:, i, 3 * h + 2:3 * h + 3], ALU.mult, [gr, gates], [gr])
                            stt(o_t[:, h * 64:(h + 1) * 64], po[:, sl * 65:sl * 65 + 64], gr[:, 0:1],
                                o_t[:, h * 64:(h + 1) * 64], ALU.mult, ALU.add, [po, gr, o_t], [o_t])
                    store("sp", o_scr[s, i * 128:(i + 1) * 128, 512:1024], dbuf(f"o{s}_{i}"), o_t, o_t[:], och[i % 2])
                em.barrier()

        def ln_affine(r, lng, lnb, xo, stats, mv, rstd, nb):
            for c2 in range(2):
                em.op("dve", lambda e, c2=c2: e.bn_stats(out=stats[:, c2, :], in_=r[:, c2 * 512:(c2 + 1) * 512]),
                      [r], [stats])
            em.op("dve", lambda e: e.bn_aggr(out=mv[:], in_=stats[:].rearrange("p a b -> p (a b)")), [stats], [mv])
            yield
            act(rstd[:], mv[:, 1:2], AF.Sqrt, [mv], [rstd], bias=LN_EPS, scale=1.0)
            yield
            recip(rstd[:], rstd[:], [rstd], [rstd])
            stt(nb[:], mv[:, 0:1], -1.0, rstd[:], ALU.mult, ALU.mult, [mv, rstd], [nb])
            yield
            act(xo[:], r[:], AF.Identity, [r, rstd, nb], [xo], bias=nb[:, 0:1], scale=rstd[:, 0:1])
            yield
            tt("dve", xo[:], xo[:], lng[:], ALU.mult, [xo, lng], [xo])
            yield
            tt("pool", xo[:], xo[:], lnb[:], ALU.add, [xo, lnb], [xo])
            yield

        def wout_stage(s, l, xsrc_ap, xsrc_buf, modT, h2T):
            with ExitStack() as st:
                ch = None
                wo = S(st, "wo", [128, 8, 1024], BF16)
                for c in range(8):
                    load("pool", wo, wo[:, c, :], w_out[l, c * 128:(c + 1) * 128, :], ch)
                G1 = S(st, "G1", [128, 1024])
                make_gate_bc(modT, 16, G1, st)
                lng = S(st, "lng1", [128, 1024])
                lnb = S(st, "lnb1", [128, 1024])
                load("sp", lng, lng[:], ln1_g[l:l + 1, :].to_broadcast([128, 1024]), ch)
                load("sp", lnb, lnb[:], ln1_b[l:l + 1, :].to_broadcast([128, 1024]), ch)
                W = 4
                otl = [S(st, f"otl{i}", [128, 1024], BF16) for i in range(W)]
                xt = [S(st, f"xtl{i}", [128, 1024]) for i in range(W)]
                lch = [em.new_chan(16, "wol") for _ in range(W)]
                lchx = [em.new_chan(16, "wolx") for _ in range(W)]
                oTt = [S(st, f"oTt{i}", [128, 8, 128], BF16) for i in range(W)]
                r_ = [S(st, f"r1{i}", [128, 1024]) for i in range(W)]
                xo = [S(st, f"xo{i}", [128, 1024]) for i in range(W)]
                sch = [em.new_chan(16, "wos") for _ in range(W)]
                stats_ = [S(st, f"stw{i}", [128, 2, 6]) for i in range(W)]
                mv_ = [S(st, f"mvw{i}", [128, 2]) for i in range(W)]
                rstd_ = [S(st, f"rstdw{i}", [128, 1]) for i in range(W)]
                nb_ = [S(st, f"nbw{i}", [128, 1]) for i in range(W)]
                hn_ = [S(st, f"hnw{i}", [128, 1024], BF16) for i in range(W)]

                def gen(i):
                    k = i % W
                    o_t, x_t, r, x_o = otl[k], xt[k], r_[k], xo[k]
                    load("sp", o_t, o_t[:], o_scr[s, i * 128:(i + 1) * 128, :], lch[k], src_buf=dbuf(f"o{s}_{i}"))
                    load("sp", x_t, x_t[:], xsrc_ap[i * 128:(i + 1) * 128, :], lchx[k], src_buf=dbuf(f"{xsrc_buf}_{i}"))
                    yield
                    psT = BfView(BK[2 * k])
                    for c in range(8):
                        tr(psT[:, c, :], o_t[:, c * 128:(c + 1) * 128], [o_t, identb], [psT], identb[:])
                    yield
                    cp("act", oTt[k][:], psT[:], [psT], [oTt[k]])
                    yield
                    for dh in range(2):
                        ps = BK[2 * k + 1]
                        for c in range(8):
                            mm(ps[:], oTt[k][:, c, :], wo[:, c, dh * 512:(dh + 1) * 512], c == 0, c == 7, [oTt[k], wo], [ps])
                        yield
                        tt("dve", r[:, dh * 512:(dh + 1) * 512], ps[:], G1[:, dh * 512:(dh + 1) * 512], ALU.mult,
                           [ps, G1], [r])
                        yield
                    stt(r[:], x_t[:], ALPHA, r[:], ALU.mult, ALU.add, [x_t, r], [r])
                    yield
                    yield from ln_affine(r, lng, lnb, x_o, stats_[k], mv_[k], rstd_[k], nb_[k])
                    store("sp", xa[s, i * 128:(i + 1) * 128, :], dbuf(f"xa{s}_{i}"), x_o, x_o[:], sch[k])
                    yield
                    yield from ln_tile_to_hT(x_o, modT, 24, 32, h2T, i, hn_[k], stats_[k], mv_[k], rstd_[k], psT)
                interleave([gen(i) for i in range(NT)], W)
                em.barrier()

        def moe_stage(s, l, modT, h2T, xdst_ap, xdst_buf):
            with ExitStack() as st:
                ch = None
                sele = S(st, "sele", [128, 2048], BF16)
                load("pool", sele, sele[:], k_sele, ch)
                rw = S(st, "rw", [128, 8, 16], BF16)
                load("pool", rw, rw[:], router_w.rearrange("(c p) n -> p c n", p=128), ch)
                rb = S(st, "rb", [128, 16])
                load("sp", rb, rb[:], router_bias.rearrange("(a n) -> a n", a=1).to_broadcast([128, 16]), ch)
                combT = S(st, "combT", [128, T_], BF16)
                memset("pool", combT[:], 0.0, [combT])
                yacc = S(st, "yacc", [128, NT, 1024])
                with ExitStack() as sr:
                    W = 4
                    mk = lambda nm, shp, dt=F32: [S(sr, f"{nm}{k}", shp, dt) for k in range(W)]
                    sc_, bi_, pr_, gsum_, gmax_, geq_ = mk("rsc", [128, 16]), mk("rbi", [128, 16]), mk("rpr", [128, 4, 6]), mk("rgs", [128, 4]), mk("rgm", [128, 1]), mk("rge", [128, 4])
                    m16_, mx8_, wsum_, cb_ = mk("rm16", [128, 16]), mk("rmx8", [128, 8]), mk("rws", [128, 1]), mk("rcb", [128, 16], BF16)

                    def genR(i):
                        k = i % W
                        sc, bi, pr, gsum, gmax, geq, m16, mx8, wsum, cb = sc_[k], bi_[k], pr_[k], gsum_[k], gmax_[k], geq_[k], m16_[k], mx8_[k], wsum_[k], cb_[k]
                        ps = BK[2 * k]
                        for c in range(8):
                            mm(ps[:, 0:16], h2T[:, c, i * 128:(i + 1) * 128], rw[:, c, :], c == 0, c == 7, [h2T, rw], [ps])
                        yield
                        act(sc[:], ps[:, 0:16], AF.Sigmoid, [ps], [sc])
                        yield
                        tt("dve", bi[:], sc[:], rb[:], ALU.add, [sc, rb], [bi])
                        b4 = bi[:].rearrange("p (g e) -> p g e", e=4)
                        tt("dve", pr[:, :, 0:3], b4[:, :, 0:3], b4[:, :, 1:4], ALU.add, [bi], [pr])
                        tt("dve", pr[:, :, 3:5], b4[:, :, 0:2], b4[:, :, 2:4], ALU.add, [bi], [pr])
                        tt("dve", pr[:, :, 5:6], b4[:, :, 0:1], b4[:, :, 3:4], ALU.add, [bi], [pr])
                        yield
                        red(gsum[:], pr[:], ALU.max, [pr], [gsum])
                        red(gmax[:], gsum[:], ALU.max, [gsum], [gmax])
                        yield
                        ts("dve", geq[:], gsum[:], gmax[:, 0:1], None, ALU.is_ge, None, [gsum, gmax], [geq])
                        ts("dve", geq[:], geq[:], 1e9, -1e9, ALU.mult, ALU.add, [geq], [geq])
                        tt("dve", m16[:].rearrange("p (g e) -> p g e", e=4), b4,
                           geq[:].unsqueeze(2).to_broadcast([128, 4, 4]), ALU.add, [bi, geq], [m16])
                        yield
                        em.op("dve", lambda e: e.max(out=mx8[:], in_=m16[:]), [m16], [mx8])
                        ts("dve", m16[:], m16[:], mx8[:, 1:2], None, ALU.is_ge, None, [m16, mx8], [m16])
                        tt("dve", m16[:], m16[:], sc[:], ALU.mult, [m16, sc], [m16])
                        yield
                        red(wsum[:], m16[:], ALU.add, [m16], [wsum])
                        recip(wsum[:], wsum[:], [wsum], [wsum])
                        ts("dve", cb[:], m16[:], wsum[:, 0:1], None, ALU.mult, None, [m16, wsum], [cb])
                        yield
                        psT = BfView(BK[2 * k + 1])
                        tr(psT[0:16, 0, :], cb[:], [cb, identb], [psT], identb[:])
                        yield
                        cp("act", combT[0:16, i * 128:(i + 1) * 128], psT[0:16, 0, :], [psT], [combT])
                        yield
                    interleave([genR(i) for i in range(NT)], W)
                    em.barrier()
                with ExitStack() as sx:
                    aT = S(sx, "aT", [128, 4, T_], BF16)
                    wgs = Rot([S(sx, f"wg{i}", [128, 8, 256], BF16) for i in range(2)])
                    wus = Rot([S(sx, f"wu{i}", [128, 8, 256], BF16) for i in range(2)])
                    wds = Rot([S(sx, f"wd{i}", [128, 2, 1024], BF16) for i in range(4)])
                    wchg = Rot([em.new_chan(16, "moewg") for _ in range(2)])
                    wchu = Rot([em.new_chan(16, "moewu") for _ in range(2)])
                    wchd = Rot([em.new_chan(16, "moewd") for _ in range(4)])
                    cbs = S(sx, "cbs", [128, 512], BF16)
                    sg = S(sx, "sg", [128, 512])
                    tu = S(sx, "tu", [128, 512])
                    for eg in range(8):
                        wd_g = []
                        for el in range(2):
                            e_ = eg * 2 + el
                            wg = wgs.next()
                            wu = wus.next()
                            wd = wds.next()
                            wcg = wchg.next()
                            wcu = wchu.next()
                            wcd = wchd.next()
                            for c4 in range(4):
                                load("pool", wg, wg[:, c4 * 2:c4 * 2 + 2, :],
                                     moe_wg[l, e_, c4 * 256:(c4 + 1) * 256, :].rearrange("(c p) n -> p c n", p=128), wcg)
                                load("pool", wu, wu[:, c4 * 2:c4 * 2 + 2, :],
                                     moe_wu[l, e_, c4 * 256:(c4 + 1) * 256, :].rearrange("(c p) n -> p c n", p=128), wcu)
                            load("pool", wd, wd[:], moe_wd[l, e_].rearrange("(c p) n -> p c n", p=128), wcd)
                            wd_g.append(wd)
                            for tg in range(4):
                                mm(psM[:], sele[:, e_ * 128:(e_ + 1) * 128], combT[:, tg * 512:(tg + 1) * 512], True, True,
                                   [sele, combT], [psM])
                                cp("act", cbs[:], psM[:], [psM], [cbs])
                                for fc in range(2):
                                    psg = PS.next()
                                    psu = PS.next()
                                    for c in range(8):
                                        mm(psg[:], wg[:, c, fc * 128:(fc + 1) * 128], h2T[:, c, tg * 512:(tg + 1) * 512],
                                           c == 0, c == 7, [wg, h2T], [psg])
                                    for c in range(8):
                                        mm(psu[:], wu[:, c, fc * 128:(fc + 1) * 128], h2T[:, c, tg * 512:(tg + 1) * 512],
                                           c == 0, c == 7, [wu, h2T], [psu])
                                    act(sg[:], psg[:], AF.Silu, [psg], [sg])
                                    tt("dve", tu[:], sg[:], psu[:], ALU.mult, [sg, psu], [tu])
                                    tt("dve", aT[:, el * 2 + fc, tg * 512:(tg + 1) * 512], tu[:], cbs[:], ALU.mult,
                                       [tu, cbs], [aT])
                        for i in range(NT):
                            for dh in range(2):
                                po = PO.next()
                                k = 0
                                for el in range(2):
                                    for fc in range(2):
                                        mm(po[:], aT[:, el * 2 + fc, i * 128:(i + 1) * 128],
                                           wd_g[el][:, fc, dh * 512:(dh + 1) * 512], k == 0, k == 3, [aT, wd_g[el]], [po])
                                        k += 1
                                if eg == 0:
                                    cp("act", yacc[:, i, dh * 512:(dh + 1) * 512], po[:], [po], [yacc])
                                else:
                                    tt("dve", yacc[:, i, dh * 512:(dh + 1) * 512], yacc[:, i, dh * 512:(dh + 1) * 512], po[:],
                                       ALU.add, [yacc, po], [yacc])
                    em.barrier()
                with ExitStack() as se:
                    G2 = S(se, "G2", [128, 1024])
                    make_gate_bc(modT, 40, G2, se)
                    lng = S(se, "lng2", [128, 1024])
                    lnb = S(se, "lnb2", [128, 1024])
                    load("sp", lng, lng[:], ln2_g[l:l + 1, :].to_broadcast([128, 1024]), ch)
                    load("sp", lnb, lnb[:], ln2_b[l:l + 1, :].to_broadcast([128, 1024]), ch)
                    W = 4
                    xt = [S(se, f"xte{i}", [128, 1024]) for i in range(W)]
                    lch = [em.new_chan(16, "mel") for _ in range(W)]
                    xo = [S(se, f"xoe{i}", [128, 1024]) for i in range(W)]
                    sch = [em.new_chan(16, "mes") for _ in range(W)]
                    r_ = [S(se, f"r2{i}", [128, 1024]) for i in range(W)]
                    stats_ = [S(se, f"ste{i}", [128, 2, 6]) for i in range(W)]
                    mv_ = [S(se, f"mve{i}", [128, 2]) for i in range(W)]
                    rstd_ = [S(se, f"rstde{i}", [128, 1]) for i in range(W)]
                    nb_ = [S(se, f"nbe{i}", [128, 1]) for i in range(W)]

                    def gen(i):
                        k = i % W
                        x_t, r, x_o = xt[k], r_[k], xo[k]
                        load("sp", x_t, x_t[:], xa[s, i * 128:(i + 1) * 128, :], lch[k], src_buf=dbuf(f"xa{s}_{i}"))
                        yield
                        tt("dve", r[:], yacc[:, i, :], G2[:], ALU.mult, [yacc, G2], [r])
                        yield
                        stt(r[:], x_t[:], ALPHA, r[:], ALU.mult, ALU.add, [x_t, r], [r])
                        yield
                        yield from ln_affine(r, lng, lnb, x_o, stats_[k], mv_[k], rstd_[k], nb_[k])
                        store("sp", xdst_ap[i * 128:(i + 1) * 128, :], dbuf(f"{xdst_buf}_{i}"), x_o, x_o[:], sch[k])
                        yield
                    interleave([gen(i) for i in range(NT)], W)
                    em.barrier()

        modA = S(gs, "modA", [128, n_layers, n_seq, 48])
        if upto >= 2:
            _tok = em.chan_mark()
            make_mod_all(modA)
            em.chan_release(_tok)
        for s in range(n_seq):
            _tok = em.chan_mark()
            make_tables(s)
            em.chan_release(_tok)
            for l in range(n_layers):
                if l == 0:
                    xsrc_ap, xsrc_buf = x_in[s], f"xin{s}"
                else:
                    xsrc_ap, xsrc_buf = xb[s], f"xb{s}"
                last = (l == n_layers - 1)
                xdst_ap, xdst_buf = (out[s], f"out{s}") if last else (xb[s], f"xb{s}")
                if upto < 2:
                    continue
                modT = ApView(modA[:, l, s, :], modA.b)
                if upto < 3:
                    continue
                with ExitStack() as sa:
                    hT = S(sa, "hT", [128, 8, T_], BF16)
                    with ExitStack() as sa2:
                        _tok = em.chan_mark()
                        ln_to_hT(xsrc_ap, xsrc_buf, modT, 0, 8, hT, sa2, "a")
                        em.barrier()
                        em.chan_release(_tok)
                    if upto >= 4:
                        _tok = em.chan_mark()
                        mla_stage(s, l, hT)
                        em.chan_release(_tok)
                    if upto >= 5:
                        _tok = em.chan_mark()
                        swa_stage(s, l, hT)
                        em.chan_release(_tok)
                    if upto >= 6:
                        _tok = em.chan_mark()
                        nsa_stage(s, l, hT)
                        em.chan_release(_tok)
                if upto < 7:
                    continue
                with ExitStack() as sb:
                    h2T = S(sb, "h2T", [128, 8, T_], BF16)
                    _tok = em.chan_mark()
                    wout_stage(s, l, xsrc_ap, xsrc_buf, modT, h2T)
                    em.chan_release(_tok)
                    if upto >= 8:
                        _tok = em.chan_mark()
                        moe_stage(s, l, modT, h2T, xdst_ap, xdst_buf)
                        em.chan_release(_tok)
            if dbg:
                dch = em.new_chan(16, "dbg")
                for i in range(NT):
                    em.dma("sp", dch, lambda e, s=s, i=i: e.dma_start(out=dbg_o[s, i * 128:(i + 1) * 128, :], in_=o_scr[s, i * 128:(i + 1) * 128, :]), [dbuf(f"o{s}_{i}")], [dbuf("dbgo")])
                    em.dma("sp", dch, lambda e, s=s, i=i: e.dma_start(out=dbg_x1[s, i * 128:(i + 1) * 128, :], in_=xa[s, i * 128:(i + 1) * 128, :]), [dbuf(f"xa{s}_{i}")], [dbuf("dbgx")])
        em.barrier()
        em.emit()
        print("ninst", em.ninst, "nsem", em.nsem, "n_inc", em.n_inc, flush=True)
    return nc


def make_consts():
    k = {}
    k["k_ident"] = np.eye(128, dtype=np.float32)
    p = np.arange(128)
    invf = np.ones((128, 2), np.float32)
    invf[:, 0] = (1.0 / (10000.0 ** ((2.0 * (p % 32)) / 64.0))).astype(np.float32)
    pm = p - 64
    invf[64:96, 1] = (1.0 / (10000.0 ** ((2.0 * (pm[64:96] % 16)) / 32.0))).astype(np.float32)
    k["k_invf"] = invf
    sgn = np.ones((128, 2), np.float32)
    sgn[:, 0] = np.where((p % 64) < 32, -1.0, 1.0)
    sgn[64:96, 1] = np.where(pm[64:96] < 16, -1.0, 1.0)
    k["k_sgn"] = sgn
    kk = np.arange(128)[:, None]
    qq = np.arange(128)[None, :]
    mdiag = np.where(kk <= qq, 0.0, NEG).astype(np.float32)
    mprev = np.where(kk > qq, 0.0, NEG).astype(np.float32)
    k["k_mdiag4"] = np.tile(mdiag, (1, 4))
    k["k_mprev4"] = np.tile(mprev, (1, 4))
    k["k_mwin4"] = np.concatenate([mprev, mprev, mdiag, mdiag], axis=1)
    t = (np.arange(NT)[None, :, None] * 128 + np.arange(128)[:, None, None])
    c = np.arange(64)[None, None, :]
    cm = np.where(c * 32 + 31 <= t, 0.0, NEG).astype(np.float32)
    k["k_cmpmask"] = np.tile(cm, (1, 1, 4)).reshape(128, NT * 256)
    tb = t // 32
    future = (c * 32 > t)
    forced = (c == 0) | (c == tb) | (c == tb - 1)
    keep = np.where(future | forced, 0.0, 1.0).astype(np.float32)
    add = np.where(future, -1e30, np.where(forced, 1e4, 0.0)).astype(np.float32)
    k["k_keep"] = keep.reshape(128, NT * 64)
    k["k_add"] = add.reshape(128, NT * 64)
    ex = np.zeros((128, 2048), np.float32)
    ex[0:64] = (np.arange(2048)[None, :] // 32 == np.arange(64)[:, None]).astype(np.float32)
    k["k_expand"] = ex
    se = np.zeros((128, 16, 128), np.float32)
    for e in range(16):
        se[e, e, :] = 1.0
    k["k_sele"] = se.reshape(128, 2048)
    return k


_NC_CACHE = {}


def kernel(**inputs):
    n_cores = 8
    if "nc" not in _NC_CACHE:
        _NC_CACHE["nc"] = build()
    nc = _NC_CACHE["nc"]
    consts = make_consts()
    shared = {k: np.ascontiguousarray(v) for k, v in inputs.items() if k not in ("x", "c", "positions")}
    in_maps = []
    for i in range(n_cores):
        m = dict(shared)
        m.update(consts)
        m["x"] = np.ascontiguousarray(inputs["x"][2 * i:2 * i + 2])
        m["c"] = np.ascontiguousarray(inputs["c"][2 * i:2 * i + 2])
        m["positions"] = np.ascontiguousarray(inputs["positions"][2 * i:2 * i + 2]).astype(np.int32)
        in_maps.append(m)
    res = run_bass_kernel_spmd(nc, in_maps, core_ids=list(range(n_cores)))
    return np.concatenate([r["out"] for r in res.results], axis=0).astype(np.float32)
```

```python
from contextlib import ExitStack
import math
import numpy as np
import concourse.bass as bass
import concourse.mybir as mybir
from concourse.bass_utils import run_bass_kernel_spmd

F32 = mybir.dt.float32
BF16 = mybir.dt.bfloat16
I32 = mybir.dt.int32
ALU = mybir.AluOpType
AF = mybir.ActivationFunctionType
AX = mybir.AxisListType

T_ = 2048
D_ = 1024
NT = 16
DEPTH = 2
ALPHA = (2 * DEPTH) ** 0.25
LN_EPS = 1e-5
RMS_EPS = 1e-6
NEG = -30000.0
SEM_EPOCH = 12000
MAX_INFLIGHT_DMA = 8
LNW = 4

O_CQ, O_CKV, O_KPE, O_SQ, O_SK, O_SV, O_NQ, O_NKC, O_NVC, O_NKS, O_NVS, O_NKW, O_NVW, O_NG = (
    0, 256, 384, 416, 672, 800, 928, 1440, 1568, 1696, 1824, 1952, 2080, 2208)


class Chan:
    __slots__ = ("sem", "count", "step", "sig", "q", "alt")

    def __init__(self, sem, step):
        self.sem = sem
        self.count = 0
        self.step = step
        self.sig = None
        self.q = None
        self.alt = None


class Buf:
    __slots__ = ("name", "w", "r", "ch")

    def __init__(self, name):
        self.name = name
        self.w = None
        self.r = {}
        self.ch = None


class Em:
    ENGS = ("pe", "act", "dve", "pool", "sp")

    def __init__(self, nc, stack):
        self.nc = nc
        self.stack = stack
        self.prog = {e: [] for e in self.ENGS}
        self.known = {e: {} for e in self.ENGS}
        self.nsem = 0
        self.all_chans = []
        self.free_dma = []
        self.live_dma = []
        self.inflight = {e: [] for e in self.ENGS}
        self.chan = {}
        for e in ("pe", "act", "dve", "pool"):
            self.chan[e] = self.new_chan(1, e)
        self.ninst = 0

    def _raw_dma_chan(self, name):
        self.nsem += 1
        sem = self.stack.enter_context(self.nc.semaphore(f"s{self.nsem}_{name}"))
        ch = Chan(sem, 16)
        self.all_chans.append(ch)
        return ch

    def new_chan(self, step=16, name="c"):
        if step == 16:
            if self.free_dma:
                ch = self.free_dma.pop()
            else:
                ch = self._raw_dma_chan(name)
            self.live_dma.append(ch)
            return ch
        self.nsem += 1
        sem = self.stack.enter_context(self.nc.semaphore(f"s{self.nsem}_{name}"))
        ch = Chan(sem, step)
        self.all_chans.append(ch)
        return ch

    def _need(self, eng, waits, dep):
        if dep is None:
            return
        ch, val = dep
        if eng == "pe" and ch is self.chan.get("pe"):
            return
        if self.known[eng].get(ch, 0) >= val:
            return
        if waits.get(ch, 0) < val:
            waits[ch] = val

    def _deps(self, eng, reads, writes, same_ch=None):
        waits = {}
        for b in reads:
            self._need(eng, waits, b.w)
        for b in writes:
            if same_ch is not None and b.w is not None and b.w[0] is same_ch and not b.r:
                continue
            self._need(eng, waits, b.w)
            for ch, val in b.r.items():
                self._need(eng, waits, (ch, val))
        for ch, val in waits.items():
            self.known[eng][ch] = val
        return list(waits.items())

    def _commit(self, ch, reads, writes):
        ch.count += ch.step
        val = ch.count
        for b in reads:
            if b.r.get(ch, 0) < val:
                b.r[ch] = val
        for b in writes:
            b.w = (ch, val)
            b.r = {}

    def op(self, eng, fn, reads=(), writes=()):
        if self.chan[eng].count >= SEM_EPOCH:
            self.chan[eng] = self.new_chan(1, eng)
        reads = [x.b if hasattr(x, "b") else x for x in reads]
        writes = [x.b if hasattr(x, "b") else x for x in writes]
        waits = self._deps(eng, reads, writes)
        ch = self.chan[eng]
        self._commit(ch, reads, writes)
        self.prog[eng].append((waits, fn, ch, ch.count))
        self.ninst += 1

    def dma(self, q, ch, fn, reads=(), writes=()):
        reads = [x.b if hasattr(x, "b") else x for x in reads]
        writes = [x.b if hasattr(x, "b") else x for x in writes]
        if ch.q is None:
            ch.q = q
        elif ch.q != q:
            if ch.alt is None:
                ch.alt = self._raw_dma_chan("alt")
                ch.alt.q = q
            ch = ch.alt
        waits = self._deps(q, reads, writes, same_ch=ch)
        sig = frozenset(id(b) for b in list(reads) + list(writes))
        if ch.count > 0 and ch.sig != sig and self.known[q].get(ch, 0) < ch.count:
            waits = [w for w in waits if w[0] is not ch] + [(ch, ch.count)]
            self.known[q][ch] = ch.count
        ch.sig = sig
        fifo = self.inflight[q]
        fifo[:] = [(c_, v_) for (c_, v_) in fifo if self.known[q].get(c_, 0) < v_]
        while len(fifo) >= MAX_INFLIGHT_DMA:
            c_, v_ = fifo.pop(0)
            if self.known[q].get(c_, 0) < v_:
                waits = [w for w in waits if not (w[0] is c_ and w[1] <= v_)] + [(c_, v_)]
                self.known[q][c_] = v_
            fifo[:] = [(c2, v2) for (c2, v2) in fifo if self.known[q].get(c2, 0) < v2]
        self._commit(ch, reads, writes)
        fifo.append((ch, ch.count))
        self.prog[q].append((waits, fn, ch, ch.count))
        self.ninst += 1

    def chan_mark(self):
        return len(self.live_dma)

    def chan_release(self, tok):
        rel = self.live_dma[tok:]
        del self.live_dma[tok:]
        self.free_dma.extend(rel)

    def barrier(self):
        for eng in self.ENGS:
            waits = []
            for ch in self.all_chans:
                if ch.count > 0 and self.known[eng].get(ch, 0) < ch.count:
                    if eng == "pe" and ch is self.chan["pe"]:
                        continue
                    waits.append((ch, ch.count))
                    self.known[eng][ch] = ch.count
            if waits:
                self.prog[eng].append((waits, None, None, 0))

    def emit(self):
        nc = self.nc
        prog = self.prog
        targets = {}
        for eng in self.ENGS:
            for waits, fn, ch, val in prog[eng]:
                for wch, wval in waits:
                    if wch.step == 1:
                        targets.setdefault(wch, set()).add(wval)
        remap = {ch: {v: i + 1 for i, v in enumerate(sorted(vals))} for ch, vals in targets.items()}
        self.n_inc = 0
        with nc.Block() as block:
            def run(engh, items):
                for waits, fn, ch, val in items:
                    for wch, wval in waits:
                        if wch.step == 1:
                            engh.wait_ge(wch.sem, remap[wch][wval])
                        else:
                            engh.wait_ge(wch.sem, wval)
                    if fn is not None:
                        ins = fn(engh)
                        if ch.step == 16:
                            ins.then_inc(ch.sem, 16)
                        elif val in remap.get(ch, ()):
                            ins.then_inc(ch.sem, 1)
                            self.n_inc += 1

            @block.tensor
            def _(e):
                run(e, prog["pe"])

            @block.scalar
            def _(e):
                run(e, prog["act"])

            @block.vector
            def _(e):
                run(e, prog["dve"])

            @block.gpsimd
            def _(e):
                run(e, prog["pool"])

            @block.sync
            def _(e):
                run(e, prog["sp"])


class TT:
    def __init__(self, em, stack, name, shape, dtype, psum=False):
        nc = em.nc
        if psum:
            self.t = stack.enter_context(nc.psum_tensor(name, list(shape), dtype))
        else:
            self.t = stack.enter_context(nc.sbuf_tensor(name, list(shape), dtype))
        self.b = Buf(name)

    def __getitem__(self, k):
        return self.t[k]


class Rot:
    def __init__(self, items):
        self.items = items
        self.i = 0

    def next(self):
        x = self.items[self.i % len(self.items)]
        self.i += 1
        return x


def build(n_seq=2, n_layers=2, dbg=False, upto=99):
    nc = bass.Bass("TRN2", target_bir_lowering=False)

    def din(name, shape, dt=F32):
        return nc.dram_tensor(name, list(shape), dt, kind="ExternalInput").ap()

    x_in = din("x", [n_seq, T_, D_])
    c_in = din("c", [n_seq, D_])
    pos_in = din("positions", [n_seq, T_], I32)
    ada_w = din("ada_w", [2, 1024, 6144])
    ada_b = din("ada_b", [2, 6144])
    w_in = din("w_in", [2, 1024, 2232])
    mla_qn = din("mla_q_norm", [2, 256])
    mla_wuq = din("mla_w_uq", [2, 256, 384])
    mla_kvn = din("mla_kv_norm", [2, 128])
    mla_wukv = din("mla_w_ukv", [2, 128, 512])
    swa_sinks = din("swa_sinks", [2, 4])
    cmp_pos = din("nsa_cmp_pos", [2, 32, 64])
    phi_k1 = din("nsa_phi_k1", [2, 2048, 256])
    phi_k2 = din("nsa_phi_k2", [2, 256, 64])
    phi_v1 = din("nsa_phi_v1", [2, 2048, 256])
    phi_v2 = din("nsa_phi_v2", [2, 256, 64])
    w_out = din("w_out", [2, 1024, 1024])
    ln1_g = din("ln1_g", [2, 1024])
    ln1_b = din("ln1_b", [2, 1024])
    ln2_g = din("ln2_g", [2, 1024])
    ln2_b = din("ln2_b", [2, 1024])
    router_w = din("router_w", [1024, 16])
    router_bias = din("router_bias", [16])
    moe_wg = din("moe_w_gate", [2, 16, 1024, 256])
    moe_wu = din("moe_w_up", [2, 16, 1024, 256])
    moe_wd = din("moe_w_down", [2, 16, 256, 1024])
    k_ident = din("k_ident", [128, 128])
    k_invf = din("k_invf", [128, 2])
    k_sgn = din("k_sgn", [128, 2])
    k_mdiag4 = din("k_mdiag4", [128, 512])
    k_mprev4 = din("k_mprev4", [128, 512])
    k_mwin4 = din("k_mwin4", [128, 512])
    k_cmpmask = din("k_cmpmask", [128, NT * 256])
    k_keep = din("k_keep", [128, NT * 64])
    k_add = din("k_add", [128, NT * 64])
    k_expand = din("k_expand", [128, 2048])
    k_sele = din("k_sele", [128, 2048])

    out = nc.dram_tensor("out", [n_seq, T_, D_], F32, kind="ExternalOutput").ap()
    xa = nc.dram_tensor("xa", [n_seq, T_, D_], F32).ap()
    xb = nc.dram_tensor("xb", [n_seq, T_, D_], F32).ap()
    o_scr = nc.dram_tensor("o_scr", [n_seq, T_, D_], BF16).ap()
    tab_scr = nc.dram_tensor("tab_scr", [n_seq, 4, 128, T_], F32).ap()
    dbg_o = None
    if dbg:
        dbg_o = nc.dram_tensor("dbg_o", [n_seq, T_, D_], BF16, kind="ExternalOutput").ap()
        dbg_x1 = nc.dram_tensor("dbg_x1", [n_seq, T_, D_], F32, kind="ExternalOutput").ap()

    with ExitStack() as gs:
        em = Em(nc, gs)
        dram_bufs = {}

        def dbuf(name):
            if name not in dram_bufs:
                dram_bufs[name] = Buf(name)
            return dram_bufs[name]

        uniq = [0]

        def S(stack, name, shape, dt=F32):
            uniq[0] += 1
            return TT(em, stack, f"{name}_{uniq[0]}", shape, dt)

        def mm(out_, lhsT, rhs, start, stop, R, W):
            em.op("pe", lambda e: e.matmul(out_, lhsT=lhsT, rhs=rhs, start=start, stop=stop), R, W)

        def tr(out_, in_, R, W, ident):
            em.op("pe", lambda e: e.transpose(out=out_, in_=in_, identity=ident), R, W)

        def act(out_, in_, func, R, W, bias=0.0, scale=1.0, accum=None):
            if accum is None:
                em.op("act", lambda e: e.activation(out=out_, in_=in_, func=func, bias=bias, scale=scale), R, W)
            else:
                em.op("act", lambda e: e.activation(out=out_, in_=in_, func=func, bias=bias, scale=scale,
                                                    accum_out=accum), R, W)

        def cp(eng, out_, in_, R, W):
            if eng == "act":
                em.op("act", lambda e: e.copy(out=out_, in_=in_), R, W)
            else:
                em.op(eng, lambda e: e.tensor_copy(out=out_, in_=in_), R, W)

        def tt(eng, out_, a, b, op, R, W):
            em.op(eng, lambda e: e.tensor_tensor(out=out_, in0=a, in1=b, op=op), R, W)

        def ts(eng, out_, a, s1, s2, op0, op1, R, W):
            if s2 is None:
                em.op(eng, lambda e: e.tensor_scalar(out=out_, in0=a, scalar1=s1, scalar2=None, op0=op0), R, W)
            else:
                em.op(eng, lambda e: e.tensor_scalar(out=out_, in0=a, scalar1=s1, scalar2=s2, op0=op0, op1=op1), R, W)

        def stt(out_, in0, scalar, in1, op0, op1, R, W):
            em.op("dve", lambda e: e.scalar_tensor_tensor(out=out_, in0=in0, scalar=scalar, in1=in1, op0=op0, op1=op1), R, W)

        def red(out_, in_, op, R, W, axis=AX.X):
            em.op("dve", lambda e: e.tensor_reduce(out=out_, in_=in_, axis=axis, op=op), R, W)

        def recip(out_, in_, R, W):
            em.op("dve", lambda e: e.reciprocal(out=out_, in_=in_), R, W)

        def memset(eng, out_, val, W):
            em.op(eng, lambda e: e.memset(out_, val), (), W)

        def load(q, dst_t, dst_ap, src_ap, ch, src_buf=None, slow=False):
            if ch is None:
                if dst_t.b.ch is None:
                    dst_t.b.ch = em.new_chan(16, "ld")
                ch = dst_t.b.ch
            R = [src_buf] if src_buf is not None else []
            if slow:
                em.dma(q, ch, lambda e: e.dma_start(out=dst_ap, in_=src_ap, allow_slow_non_contiguous=True), R, [dst_t])
            else:
                em.dma(q, ch, lambda e: e.dma_start(out=dst_ap, in_=src_ap), R, [dst_t])

        def store(q, dst_ap, dst_buf, src_t, src_ap, ch):
            em.dma(q, ch, lambda e: e.dma_start(out=dst_ap, in_=src_ap), [src_t], [dst_buf])

        identb = S(gs, "identb", [128, 128], BF16)
        onesb = S(gs, "onesb", [128, 128], BF16)
        zerosb = S(gs, "zerosb", [128, 512], BF16)
        mdiag4 = S(gs, "mdiag4", [128, 512], BF16)
        mprev4 = S(gs, "mprev4", [128, 512], BF16)
        mwin4 = S(gs, "mwin4", [128, 512], BF16)
        invf = S(gs, "invf", [128, 2])
        sgn = S(gs, "sgn", [128, 2])
        cch = None
        load("pool", identb, identb[:], k_ident, cch)
        load("pool", mdiag4, mdiag4[:], k_mdiag4, cch)
        load("pool", mprev4, mprev4[:], k_mprev4, cch)
        load("pool", mwin4, mwin4[:], k_mwin4, cch)
        load("sp", invf, invf[:], k_invf, cch)
        load("sp", sgn, sgn[:], k_sgn, cch)
        memset("dve", onesb[:], 1.0, [onesb])
        memset("dve", zerosb[:], 0.0, [zerosb])

        BK = [TT(em, gs, f"bk{i}", [128, 512], F32, psum=True) for i in range(8)]

        class BfView:
            def __init__(self, bank):
                self.t = bank[:].bitcast(BF16).rearrange("p (c n) -> p c n", n=128)
                self.b = bank.b

            def __getitem__(self, k):
                return self.t[k]

        PS = Rot(BK[0:5])
        PT = Rot([BfView(BK[3]), BfView(BK[4])])

        def interleave(gens, width):
            pending = list(gens)
            active = [pending.pop(0) for _ in range(min(width, len(pending)))]
            while active:
                for g_ in list(active):
                    try:
                        next(g_)
                    except StopIteration:
                        active.remove(g_)
                        if pending:
                            active.append(pending.pop(0))
        PO = Rot(BK[5:7])
        psM = BK[7]

        def make_tables(s):
            with ExitStack() as st:
                posi = S(st, "posi", [128, T_], I32)
                ang = S(st, "ang", [128, T_])
                y = S(st, "ty", [128, T_])
                ki = S(st, "tki", [128, T_], I32)
                kf = S(st, "tkf", [128, T_])
                r = S(st, "tr", [128, T_])
                m = S(st, "tm", [128, T_])
                res = S(st, "tres", [128, T_])
                ch = em.new_chan(16, "tab")
                load("sp", posi, posi[:], pos_in[s:s + 1, :].to_broadcast([128, T_]), ch)
                cp("dve", ang[:], posi[:], [posi], [ang])
                for lay in range(2):
                    for kind in range(2):
                        ts("dve", y[:], ang[:], invf[:, lay:lay + 1], (math.pi / 2 if kind == 0 else 0.0),
                           ALU.mult, ALU.add, [ang, invf], [y])
                        ts("dve", r[:], y[:], 1.0 / (2 * math.pi), None, ALU.mult, None, [y], [r])
                        cp("dve", ki[:], r[:], [r], [ki])
                        cp("dve", kf[:], ki[:], [ki], [kf])
                        stt(r[:], kf[:], -6.28125, y[:], ALU.mult, ALU.add, [kf, y], [r])
                        stt(r[:], kf[:], -0.0019353071795864769, r[:], ALU.mult, ALU.add, [kf, r], [r])
                        ts("dve", m[:], r[:], math.pi, None, ALU.is_gt, None, [r], [m])
                        stt(r[:], m[:], -2 * math.pi, r[:], ALU.mult, ALU.add, [m, r], [r])
                        ts("dve", m[:], r[:], -math.pi, None, ALU.is_lt, None, [r], [m])
                        stt(r[:], m[:], 2 * math.pi, r[:], ALU.mult, ALU.add, [m, r], [r])
                        ts("dve", r[:], r[:], math.pi, -math.pi, ALU.min, ALU.max, [r], [r])
                        act(res[:], r[:], AF.Sin, [r], [res])
                        if kind == 1:
                            ts("dve", res[:], res[:], sgn[:, lay:lay + 1], None, ALU.mult, None, [res, sgn], [res])
                        store("sp", tab_scr[s, lay * 2 + kind], dbuf(f"tab{s}"), res, res[:], ch)
                em.barrier()

        class ApView:
            def __init__(self, ap, buf):
                self.t = ap
                self.b = buf

            def __getitem__(self, k):
                return self.t[k]

        def make_mod_all(modA):
            for l in range(n_layers):
                with ExitStack() as st:
                    craw = S(st, "craw", [128, 8, n_seq])
                    cs = S(st, "cs", [128, 8, n_seq], BF16)
                    abT = S(st, "abT", [128, 48])
                    ch = em.new_chan(16, "mod")
                    for s_ in range(n_seq):
                        load("sp", craw, craw[:, :, s_], c_in[s_].rearrange("(j p) -> p j", p=128), ch, slow=True)
                    ch2 = em.new_chan(16, "mod2")
                    load("sp", abT, abT[:], ada_b[l].rearrange("(j p) -> p j", p=128), ch2, slow=True)
                    act(cs[:], craw[:], AF.Silu, [craw], [cs])
                    wb = Rot([S(st, f"adaw{i}", [128, 8, 1024], BF16) for i in range(2)])
                    wch = [em.new_chan(16, "adaw") for _ in range(2)]
                    for blk in range(6):
                        w = wb.next()
                        for c in range(8):
                            load("pool", w, w[:, c, :], ada_w[l, c * 128:(c + 1) * 128, blk * 1024:(blk + 1) * 1024], wch[blk % 2])
                        for j in range(8):
                            col = blk * 8 + j
                            for c in range(8):
                                mm(psM[:, col * n_seq:(col + 1) * n_seq], w[:, c, j * 128:(j + 1) * 128], cs[:, c, :], c == 0, c == 7,
                                   [w, cs], [psM])
                    pv = psM[:, 0:48 * n_seq].rearrange("p (c s) -> p c s", s=n_seq)
                    for s_ in range(n_seq):
                        tt("dve", modA[:, l, s_, :], pv[:, :, s_], abT[:], ALU.add, [psM, abT], [modA])
                        for a in (8, 32):
                            ts("dve", modA[:, l, s_, a:a + 8], modA[:, l, s_, a:a + 8], 1.0, None, ALU.add, None, [modA], [modA])
                    em.barrier()

        def make_gate_bc(modT, col0, G, st):
            dg = S(st, "dg", [128, 128], BF16)
            for j in range(8):
                ts("dve", dg[:], identb[:], modT[:, col0 + j:col0 + j + 1], None, ALU.mult, None, [identb, modT], [dg])
                ps = PS.next()
                mm(ps[:, 0:128], onesb[:], dg[:], True, True, [onesb, dg], [ps])
                act(G[:, j * 128:(j + 1) * 128], ps[:, 0:128], AF.Identity, [ps], [G], bias=1.0, scale=1.0)

        def ln_to_hT(xsrc_ap, xsrc_buf, modT, sh_col, sc_col, hT, st, tag):
            W = LNW
            xt = [S(st, f"xt{tag}{i}", [128, 1024]) for i in range(W)]
            xch = [em.new_chan(16, "xt") for _ in range(W)]
            hn = [S(st, f"hn{tag}{i}", [128, 1024], BF16) for i in range(W)]
            stats = [S(st, f"st{tag}{i}", [128, 2, 6]) for i in range(W)]
            mv = [S(st, f"mv{tag}{i}", [128, 2]) for i in range(W)]
            rstd = [S(st, f"rstd{tag}{i}", [128, 1]) for i in range(W)]

            def gen(i):
                k = i % W
                load("sp", xt[k], xt[k][:], xsrc_ap[i * 128:(i + 1) * 128, :], xch[k], src_buf=dbuf(f"{xsrc_buf}_{i}"))
                yield
                yield from ln_tile_to_hT(xt[k], modT, sh_col, sc_col, hT, i, hn[k], stats[k], mv[k], rstd[k], psTs[k])
            psTs = [BfView(BK[k]) for k in range(W)]
            interleave([gen(i) for i in range(NT)], W)

        def ln_tile_to_hT(x_t, modT, sh_col, sc_col, hT, i, hn, stats, mv, rstd, psT):
            for c2 in range(2):
                em.op("dve", lambda e, c2=c2: e.bn_stats(out=stats[:, c2, :], in_=x_t[:, c2 * 512:(c2 + 1) * 512]),
                      [x_t], [stats])
            em.op("dve", lambda e: e.bn_aggr(out=mv[:], in_=stats[:].rearrange("p a b -> p (a b)")), [stats], [mv])
            yield
            act(rstd[:], mv[:, 1:2], AF.Sqrt, [mv], [rstd], bias=LN_EPS, scale=1.0)
            yield
            recip(rstd[:], rstd[:], [rstd], [rstd])
            ts("dve", hn[:], x_t[:], mv[:, 0:1], rstd[:], ALU.subtract, ALU.mult, [x_t, mv, rstd], [hn])
            yield
            for c in range(8):
                tr(psT[:, c, :], hn[:, c * 128:(c + 1) * 128], [hn, identb], [psT], identb[:])
            yield
            for c in range(8):
                act(hT[:, c, i * 128:(i + 1) * 128], psT[:, c, :], AF.Identity, [psT, modT], [hT],
                    bias=modT[:, sh_col + c:sh_col + c + 1], scale=modT[:, sc_col + c:sc_col + c + 1])
            yield

        def load_w_cols(st_w, name, l, col0, ncols, chs):
            w = S(st_w, name, [128, 8, ncols], BF16)
            for c in range(8):
                load("pool", w, w[:, c, :], w_in[l, c * 128:(c + 1) * 128, col0:col0 + ncols], chs)
            return w

        def make_rot(st_w, name, w, nblk, half, eng="dve"):
            wr = S(st_w, name, [128, 8, nblk * 2 * half], BF16)
            wv = w[:].rearrange("p c (b t h) -> p c b t h", t=2, h=half)
            rv = wr[:].rearrange("p c (b t h) -> p c b t h", t=2, h=half)
            for c in range(8):
                cp(eng, rv[:, c, :, 0, :], wv[:, c, :, 1, :], [w], [wr])
                cp(eng, rv[:, c, :, 1, :], wv[:, c, :, 0, :], [w], [wr])
            return wr

        def proj_fm(ps, w, c0, c1, hT, tg, nk=8):
            for c in range(nk):
                mm(ps[0:c1 - c0, :], w[:, c, c0:c1], hT[:, c, tg * 512:(tg + 1) * 512], c == 0, c == nk - 1,
                   [w, hT], [ps])

        def rope_evac(dst_ap, dst_t, psm, psr, cosT, sinT, rows, tg, scale, tmp1, tmp2, extra=None, dsts=None):
            r0, r1 = rows
            sl = slice(tg * 512, (tg + 1) * 512)
            stt(tmp1[r0:r1, :], psm[r0:r1, :], scale, cosT[r0:r1, sl], ALU.mult, ALU.mult, [psm, cosT], [tmp1])
            stt(tmp2[r0:r1, :], psr[r0:r1, :], scale, sinT[r0:r1, sl], ALU.mult, ALU.mult, [psr, sinT], [tmp2])
            if dsts is None:
                dsts = [(r0, r1, dst_ap)]
            if extra is not None:
                tt("dve", tmp1[r0:r1, :], tmp1[r0:r1, :], tmp2[r0:r1, :], ALU.add, [tmp1, tmp2], [tmp1])
                for (a, b, ap) in dsts:
                    tt("dve", ap, tmp1[a:b, :], extra[0][a:b, :], ALU.add, [tmp1, extra[1]], [dst_t])
            else:
                for (a, b, ap) in dsts:
                    tt("dve", ap, tmp1[a:b, :], tmp2[a:b, :], ALU.add, [tmp1, tmp2], [dst_t])

        def sumsq_max(src_ap, src_t, rows, statc, col, sq, scale=1.0):
            if isinstance(sq, Rot):
                sq = sq.next()
            tt("pool", sq[0:rows, :], src_ap, src_ap, ALU.mult, [src_t], [sq])
            ps = PS.next()
            mm(ps[:], onesb[0:rows, :], sq[0:rows, :], True, True, [onesb, sq], [ps])
            red(statc[:, col:col + 1], ps[:], ALU.max, [ps], [statc])

        def finish_negC(negC, statq, nq, statk, nk, kfac, st):
            qm = S(st, "qm" + negC.b.name, [128, 1])
            km = S(st, "km" + negC.b.name, [128, 1])
            red(qm[:], statq[:, 0:nq], ALU.max, [statq], [qm])
            red(km[:], statk[:, 0:nk], ALU.max, [statk], [km])
            tt("dve", qm[:], qm[:], km[:], ALU.mult, [qm, km], [qm])
            act(km[:], qm[:], AF.Sqrt, [qm], [km], scale=kfac * 1.1025)
            ts("dve", negC[:], km[:], -1.0, None, ALU.mult, None, [km], [negC])

        def mla_stage(s, l, hT):
            sc_mla = 1.0 / math.sqrt(96.0)
            with ExitStack() as st:
                ch = None
                cosM = S(st, "cosM", [96, T_])
                sinM = S(st, "sinM", [96, T_])
                load("sp", cosM, cosM[64:96, :], tab_scr[s, 2, 64:96, :], ch, src_buf=dbuf(f"tab{s}"))
                load("sp", sinM, sinM[64:96, :], tab_scr[s, 3, 64:96, :], ch, src_buf=dbuf(f"tab{s}"))
                QT = S(st, "QTm", [96, 4, T_], BF16)
                KT = S(st, "KTm", [96, 4, T_], BF16)
                V = S(st, "Vm", [128, NT, 4, 65], BF16)
                memset("pool", V[:], 1.0, [V])
                negC = S(st, "negCm", [128, 1])
                with ExitStack() as sw:
                    wcq = load_w_cols(sw, "wcq", l, O_CQ, 256, ch)
                    wckv = load_w_cols(sw, "wckv", l, O_CKV, 128, ch)
                    wkpe = S(sw, "wkpe", [128, 8, 96], BF16)
                    wkper = S(sw, "wkper", [128, 8, 96], BF16)
                    memset("pool", wkpe[:], 0.0, [wkpe])
                    memset("pool", wkper[:], 0.0, [wkper])
                    wsrc = w_in[l].rearrange("(c p) n -> p c n", p=128)
                    for c in range(8):
                        load("pool", wkpe, wkpe[:, c, 64:96], wsrc[:, c, O_KPE:O_KPE + 32], ch)
                        load("pool", wkper, wkper[:, c, 64:80], wsrc[:, c, O_KPE + 16:O_KPE + 32], ch)
                        load("pool", wkper, wkper[:, c, 80:96], wsrc[:, c, O_KPE:O_KPE + 16], ch)
                    qn = S(sw, "qn", [128, 2])
                    kvn = S(sw, "kvn", [128, 1])
                    load("sp", qn, qn[:], mla_qn[l].rearrange("(c p) -> p c", p=128), ch, slow=True)
                    load("sp", kvn, kvn[:], mla_kvn[l].rearrange("(c p) -> p c", p=128), ch, slow=True)
                    wuq = S(sw, "wuq", [128, 2, 384], BF16)
                    wuqr = S(sw, "wuqr", [128, 2, 384], BF16)
                    load("pool", wuq, wuq[:], mla_wuq[l].rearrange("(c p) n -> p c n", p=128), ch)
                    for c in range(2):
                        ts("dve", wuq[:, c, :], wuq[:, c, :], qn[:, c:c + 1], None, ALU.mult, None, [wuq, qn], [wuq])
                    cp("dve", wuqr[:], wuq[:], [wuq], [wuqr])
                    wv4 = wuq[:].rearrange("p c (h d) -> p c h d", d=96)
                    rv4 = wuqr[:].rearrange("p c (h d) -> p c h d", d=96)
                    for c in range(2):
                        cp("dve", rv4[:, c, :, 64:80], wv4[:, c, :, 80:96], [wuq], [wuqr])
                        cp("dve", rv4[:, c, :, 80:96], wv4[:, c, :, 64:80], [wuq], [wuqr])
                    wukv = S(sw, "wukv", [128, 512], BF16)
                    load("pool", wukv, wukv[:], mla_wukv[l], ch)
                    ts("dve", wukv[:], wukv[:], kvn[:, 0:1], None, ALU.mult, None, [wukv, kvn], [wukv])
                    cqf = S(sw, "cqf", [128, 2, 512])
                    sq = S(sw, "sqm", [128, 2, 512], BF16)
                    Rq = S(sw, "Rq", [128, 512])
                    cqn = S(sw, "cqn", [128, 2, 512], BF16)
                    ckvf = S(sw, "ckvf", [128, 512])
                    ckvn = S(sw, "ckvn", [128, 512], BF16)
                    t1 = S(sw, "t1m", [128, 512])
                    t2 = S(sw, "t2m", [128, 512])
                    kpe = S(sw, "kpe", [96, 512], BF16)
                    for tg in range(4):
                        for hc in range(2):
                            ps = PS.next()
                            proj_fm(ps, wcq, hc * 128, (hc + 1) * 128, hT, tg)
                            cp("act", cqf[:, hc, :], ps[:], [ps], [cqf])
                            act(sq[:, hc, :], ps[:], AF.Square, [ps], [sq])
                        ps = PS.next()
                        for hc in range(2):
                            mm(ps[:], onesb[:], sq[:, hc, :], hc == 0, hc == 1, [onesb, sq], [ps])
                        act(Rq[:], ps[:], AF.Sqrt, [ps], [Rq], bias=RMS_EPS, scale=1.0 / 256.0)
                        recip(Rq[:], Rq[:], [Rq], [Rq])
                        for hc in range(2):
                            tt("dve", cqn[:, hc, :], cqf[:, hc, :], Rq[:], ALU.mult, [cqf, Rq], [cqn])
                        ps = PS.next()
                        proj_fm(ps, wckv, 0, 128, hT, tg)
                        cp("act", ckvf[:], ps[:], [ps], [ckvf])
                        act(sq[:, 0, :], ps[:], AF.Square, [ps], [sq])
                        ps = PS.next()
                        mm(ps[:], onesb[:], sq[:, 0, :], True, True, [onesb, sq], [ps])
                        act(Rq[:], ps[:], AF.Sqrt, [ps], [Rq], bias=RMS_EPS, scale=1.0 / 128.0)
                        recip(Rq[:], Rq[:], [Rq], [Rq])
                        tt("dve", ckvn[:], ckvf[:], Rq[:], ALU.mult, [ckvf, Rq], [ckvn])
                        for h in range(4):
                            psq = PS.next()
                            psr = PS.next()
                            for c in range(2):
                                mm(psq[0:96, :], wuq[:, c, h * 96:(h + 1) * 96], cqn[:, c, :], c == 0, c == 1,
                                   [wuq, cqn], [psq])
                            for c in range(2):
                                mm(psr[0:96, :], wuqr[:, c, h * 96:(h + 1) * 96], cqn[:, c, :], c == 0, c == 1,
                                   [wuqr, cqn], [psr])
                            act(QT[0:64, h, tg * 512:(tg + 1) * 512], psq[0:64, :], AF.Copy, [psq], [QT], scale=sc_mla)
                            rope_evac(QT[64:96, h, tg * 512:(tg + 1) * 512], QT, psq, psr, cosM, sinM, (64, 96), tg,
                                      sc_mla, t1, t2)
                        for h in range(4):
                            ps = PS.next()
                            mm(ps[0:64, :], wukv[:, h * 128:h * 128 + 64], ckvn[:], True, True, [wukv, ckvn], [ps])
                            cp("act", KT[0:64, h, tg * 512:(tg + 1) * 512], ps[0:64, :], [ps], [KT])
                        psk = PS.next()
                        psr = PS.next()
                        proj_fm(psk, wkpe, 0, 96, hT, tg)
                        proj_fm(psr, wkper, 0, 96, hT, tg)
                        rope_evac(kpe[64:96, :], kpe, psk, psr, cosM, sinM, (64, 96), tg, 1.0, t1, t2)
                        for h in range(4):
                            cp("act", KT[64:96, h, tg * 512:(tg + 1) * 512], kpe[64:96, :], [kpe], [KT])
                        wv = wukv[:].rearrange("p (h d) -> p h d", d=128)
                        for tt_ in range(4):
                            ps = PS.next()
                            mm(ps[:, 0:256].rearrange("p (h d) -> p h d", d=64), ckvn[:, tt_ * 128:(tt_ + 1) * 128],
                               wv[:, :, 64:128], True, True, [ckvn, wukv], [ps])
                            cp("act", V[:, tg * 4 + tt_, :, 0:64], ps[:, 0:256].rearrange("p (h d) -> p h d", d=64),
                               [ps], [V])
                    statq = S(sw, "statqm", [128, 16])
                    statk = S(sw, "statkm", [128, 16])
                    sqn = Rot([S(sw, f"sqnm{i}", [128, 512], BF16) for i in range(3)])
                    for h in range(4):
                        for tg in range(4):
                            sumsq_max(QT[0:96, h, tg * 512:(tg + 1) * 512], QT, 96, statq, h * 4 + tg, sqn)
                            sumsq_max(KT[0:96, h, tg * 512:(tg + 1) * 512], KT, 96, statk, h * 4 + tg, sqn)
                    finish_negC(negC, statq, 16, statk, 16, 1.0, sw)
                    em.barrier()
                pT = Rot([S(st, f"pTm{i}", [128, 512], BF16) for i in range(3)])
                ot = Rot([S(st, f"otm{i}", [128, 256], BF16) for i in range(2)])
                rden = S(st, "rdenm", [128, 1])
                och = [em.new_chan(16, "om") for _ in range(2)]
                for i in range(NT):
                    o_t = ot.next()
                    for h in range(4):
                        po = PO.next()
                        chunks = [list(range(j0, min(j0 + 4, i + 1))) for j0 in range(0, i + 1, 4)]
                        pend = None
                        for ci in range(len(chunks) + 1):
                            cur = None
                            if ci < len(chunks):
                                js = chunks[ci]
                                n = len(js) * 128
                                ps = PS.next()
                                mm(ps[:, 0:n], identb[:], zerosb[:, 0:n], True, False, [identb, zerosb], [ps])
                                for jj, j in enumerate(js):
                                    mm(ps[:, jj * 128:(jj + 1) * 128], KT[0:96, h, j * 128:(j + 1) * 128],
                                       QT[0:96, h, i * 128:(i + 1) * 128], False, (jj == len(js) - 1 and j != i), [KT, QT], [ps])
                                    if j == i:
                                        mm(ps[:, jj * 128:(jj + 1) * 128], identb[:], mdiag4[:, 0:128], False, True,
                                           [identb, mdiag4], [ps])
                                p_t = pT.next()
                                act(p_t[:, 0:n], ps[:, 0:n], AF.Exp, [ps, negC], [p_t], bias=negC[:, 0:1], scale=1.0)
                                cur = (js, p_t)
                            if pend is not None:
                                jsp, p_p = pend
                                for jj, j in enumerate(jsp):
                                    mm(po[:, 0:65], p_p[:, jj * 128:(jj + 1) * 128], V[:, j, h, :], j == 0, j == i,
                                       [p_p, V], [po])
                            pend = cur
                        recip(rden[:], po[:, 64:65], [po], [rden])
                        ts("dve", o_t[:, h * 64:(h + 1) * 64], po[:, 0:64], rden[:, 0:1], None, ALU.mult, None,
                           [po, rden], [o_t])
                    store("sp", o_scr[s, i * 128:(i + 1) * 128, 0:256], dbuf(f"o{s}_{i}"), o_t, o_t[:], och[i % 2])
                em.barrier()

        def swa_stage(s, l, hT):
            with ExitStack() as st:
                ch = None
                cosH = S(st, "cosH", [128, T_])
                sinH = S(st, "sinH", [128, T_])
                load("sp", cosH, cosH[:], tab_scr[s, 0], ch, src_buf=dbuf(f"tab{s}"))
                load("sp", sinH, sinH[:], tab_scr[s, 1], ch, src_buf=dbuf(f"tab{s}"))
                QT = S(st, "QTs", [128, 2, T_], BF16)
                KT = S(st, "KTs", [128, 2, 2, T_], BF16)
                memset("pool", KT[:], 0.0, [KT])
                V = S(st, "Vs", [128, NT, 2, 65], BF16)
                memset("pool", V[:], 1.0, [V])
                negC = S(st, "negCs", [128, 1])
                sink = S(st, "sink", [128, 4])
                sinke = S(st, "sinke", [128, 4])
                load("sp", sink, sink[:], swa_sinks[l:l + 1, :].to_broadcast([128, 4]), ch)
                with ExitStack() as sw:
                    wq = load_w_cols(sw, "wsq", l, O_SQ, 256, ch)
                    wqr = make_rot(sw, "wsqr", wq, 4, 32)
                    wk1 = load_w_cols(sw, "wsk", l, O_SK, 128, ch)
                    wk = S(sw, "wskd", [128, 8, 2, 128], BF16)
                    for g in range(2):
                        for hf in range(2):
                            cp("dve", wk[:, :, g, hf * 64:(hf + 1) * 64], wk1[:, :, g * 64:(g + 1) * 64], [wk1], [wk])
                    wkf = S(sw, "wskdf", [128, 8, 256], BF16)
                    cp("dve", wkf[:], wk[:].rearrange("p c g d -> p c (g d)"), [wk], [wkf])
                    wkr = make_rot(sw, "wskr", wkf, 4, 32)
                    wv = load_w_cols(sw, "wsv", l, O_SV, 128, ch)
                    t1 = S(sw, "t1s", [128, 512])
                    t2 = S(sw, "t2s", [128, 512])
                    for tg in range(4):
                        for p in range(2):
                            psm = PS.next()
                            psr = PS.next()
                            proj_fm(psm, wq, p * 128, (p + 1) * 128, hT, tg)
                            proj_fm(psr, wqr, p * 128, (p + 1) * 128, hT, tg)
                            rope_evac(QT[:, p, tg * 512:(tg + 1) * 512], QT, psm, psr, cosH, sinH, (0, 128), tg,
                                      0.125, t1, t2)
                        for g in range(2):
                            psm = PS.next()
                            psr = PS.next()
                            proj_fm(psm, wkf, g * 128, (g + 1) * 128, hT, tg)
                            proj_fm(psr, wkr, g * 128, (g + 1) * 128, hT, tg)
                            rope_evac(None, KT, psm, psr, cosH, sinH, (0, 128), tg, 1.0, t1, t2,
                                      dsts=[(0, 64, KT[0:64, g, 0, tg * 512:(tg + 1) * 512]),
                                            (64, 128, KT[64:128, g, 1, tg * 512:(tg + 1) * 512])])
                    for i in range(NT):
                        ps = PS.next()
                        for c in range(8):
                            mm(ps[:, 0:128], hT[:, c, i * 128:(i + 1) * 128], wv[:, c, :], c == 0, c == 7, [hT, wv], [ps])
                        cp("act", V[:, i, :, 0:64], ps[:, 0:128].rearrange("p (g d) -> p g d", d=64), [ps], [V])
                    statq = S(sw, "statqs", [128, 8])
                    statk = S(sw, "statks", [128, 8])
                    sqn = Rot([S(sw, f"sqns{i}", [128, 512], BF16) for i in range(3)])
                    for p in range(2):
                        for tg in range(4):
                            sumsq_max(QT[:, p, tg * 512:(tg + 1) * 512], QT, 128, statq, p * 4 + tg, sqn)
                            sumsq_max(KT[:, p, 0, tg * 512:(tg + 1) * 512], KT, 128, statk, p * 4 + tg, sqn)
                    finish_negC(negC, statq, 8, statk, 8, 1.0, sw)
                    act(sinke[:], sink[:], AF.Exp, [sink, negC], [sinke], bias=negC[:, 0:1], scale=1.0)
                    em.barrier()
                pT = Rot([S(st, f"pTs{i}", [128, 512], BF16) for i in range(3)])
                ot = Rot([S(st, f"ots{i}", [128, 256], BF16) for i in range(2)])
                den = S(st, "dens", [128, 1])
                och = [em.new_chan(16, "os") for _ in range(2)]
                for i in range(NT):
                    o_t = ot.next()
                    for g in range(2):
                        js = [i - 1, i] if i > 0 else [i]
                        n = len(js) * 256
                        ps = PS.next()
                        msk = mwin4 if i > 0 else mdiag4
                        mm(ps[:, 0:n], identb[:], msk[:, 0:n], True, False, [identb, msk], [ps])
                        for jj, j in enumerate(js):
                            for hf in range(2):
                                col = jj * 256 + hf * 128
                                mm(ps[:, col:col + 128], KT[:, g, hf, j * 128:(j + 1) * 128],
                                   QT[:, g, i * 128:(i + 1) * 128], False,
                                   (jj == len(js) - 1 and hf == 1), [KT, QT], [ps])
                        p_t = pT.next()
                        act(p_t[:, 0:n], ps[:, 0:n], AF.Exp, [ps, negC], [p_t], bias=negC[:, 0:1], scale=1.0)
                        for hf in range(2):
                            h = 2 * g + hf
                            po = PO.next()
                            for jj, j in enumerate(js):
                                col = jj * 256 + hf * 128
                                mm(po[:, 0:65], p_t[:, col:col + 128], V[:, j, g, :], jj == 0, jj == len(js) - 1,
                                   [p_t, V], [po])
                            tt("dve", den[:], po[:, 64:65], sinke[:, h:h + 1], ALU.add, [po, sinke], [den])
                            recip(den[:], den[:], [den], [den])
                            ts("dve", o_t[:, h * 64:(h + 1) * 64], po[:, 0:64], den[:, 0:1], None, ALU.mult, None,
                               [po, den], [o_t])
                    store("sp", o_scr[s, i * 128:(i + 1) * 128, 256:512], dbuf(f"o{s}_{i}"), o_t, o_t[:], och[i % 2])
                em.barrier()

        def nsa_stage(s, l, hT):
            with ExitStack() as st:
                ch = None
                cosH = S(st, "cosHn", [128, T_])
                sinH = S(st, "sinHn", [128, T_])
                load("sp", cosH, cosH[:], tab_scr[s, 0], ch, src_buf=dbuf(f"tab{s}"))
                load("sp", sinH, sinH[:], tab_scr[s, 1], ch, src_buf=dbuf(f"tab{s}"))
                cmpmask = S(st, "cmpmask", [128, NT, 256], BF16)
                keepM = S(st, "keepM", [128, NT, 64], BF16)
                addM = S(st, "addM", [128, NT, 64], BF16)
                expandE = S(st, "expandE", [128, T_], BF16)
                QT = S(st, "QTn", [128, 4, T_], BF16)
                KwT = S(st, "KwT", [128, 2, 2, T_], BF16)
                KsT = S(st, "KsT", [128, 2, 2, T_], BF16)
                Vs = S(st, "Vns", [128, NT, 2, 65], BF16)
                Vw = S(st, "Vnw", [128, NT, 2, 65], BF16)
                gates = S(st, "gates", [128, NT, 24])
                KcmpT = S(st, "KcmpT", [128, 2, 2, 64], BF16)
                Vcmp = S(st, "Vcmp", [128, 2, 64], BF16)
                negCw = S(st, "negCw", [128, 1])
                negCs = S(st, "negCsl", [128, 1])
                with ExitStack() as sw:
                    KcT = S(sw, "KcT", [128, T_], BF16)
                    VcT = S(sw, "VcT", [128, T_], BF16)
                    t1 = S(sw, "t1n", [128, 512])
                    t2 = S(sw, "t2n", [128, 512])
                    cpT = S(sw, "cpT", [128, 32])
                    cpt = S(sw, "cpt", [128, 512])
                    for g in range(2):
                        load("sp", cpT, cpT[g * 64:(g + 1) * 64, :], cmp_pos[l].rearrange("i d -> d i"), ch, slow=True)
                    for b in range(16):
                        cp("dve", cpt[:, b * 32:(b + 1) * 32], cpT[:], [cpT], [cpt])
                    phi1s = [S(sw, "phi1k", [128, 32, 256], BF16), S(sw, "phi1v", [128, 32, 256], BF16)]
                    phich = [em.new_chan(16, "phik"), em.new_chan(16, "phiv")]
                    raw = {nm: S(sw, "wraw" + nm, [128, 8, nc_], BF16) for nm, nc_ in
                           (("kw", 128), ("ks", 128), ("kc", 128), ("vc", 128), ("tm", 280))}

                    def load_raw():
                        wsrc_ = w_in[l].rearrange("(c p) n -> p c n", p=128)
                        for nm, off in (("kw", O_NKW), ("ks", O_NKS), ("kc", O_NKC), ("vc", O_NVC)):
                            for c in range(8):
                                load("pool", raw[nm], raw[nm][:, c, :], wsrc_[:, c, off:off + 128], None)
                        for c in range(8):
                            load("pool", raw["tm"], raw["tm"][:, c, 0:128], wsrc_[:, c, O_NVS:O_NVS + 128], None)
                            load("pool", raw["tm"], raw["tm"][:, c, 128:256], wsrc_[:, c, O_NVW:O_NVW + 128], None)
                            load("pool", raw["tm"], raw["tm"][:, c, 256:280], wsrc_[:, c, O_NG:O_NG + 24], None)
                    with ExitStack() as sp_:
                        wq = load_w_cols(sp_, "wnq", l, O_NQ, 512, ch)
                        load_raw()
                        wqr = make_rot(sp_, "wnqr", wq, 8, 32)
                        def late_init():
                            load("pool", cmpmask, cmpmask[:], k_cmpmask.rearrange("p (a b) -> p a b", b=256), ch)
                            load("pool", keepM, keepM[:], k_keep.rearrange("p (a b) -> p a b", b=64), ch)
                            load("pool", addM, addM[:], k_add.rearrange("p (a b) -> p a b", b=64), ch)
                            load("pool", expandE, expandE[:], k_expand, ch)
                            memset("pool", KwT[:], 0.0, [KwT])
                            memset("pool", KsT[:], 0.0, [KsT])
                            memset("pool", Vs[:], 1.0, [Vs])
                            memset("pool", Vw[:], 1.0, [Vw])
                            memset("pool", KcmpT[:], 0.0, [KcmpT])
                            memset("pool", Vcmp[:], 0.0, [Vcmp])
                            for which, phi in ((0, phi_k1), (1, phi_v1)):
                                for g in range(2):
                                    for i4 in range(8):
                                        load("pool", phi1s[which], phi1s[which][g * 64:(g + 1) * 64, i4 * 4:(i4 + 1) * 4, :],
                                             phi[l, i4 * 256:(i4 + 1) * 256, :].rearrange("(i d) h -> d i h", d=64), phich[which])
                        for tg in range(4):
                            for p in range(4):
                                psm = PS.next()
                                psr = PS.next()
                                proj_fm(psm, wq, p * 128, (p + 1) * 128, hT, tg)
                                proj_fm(psr, wqr, p * 128, (p + 1) * 128, hT, tg)
                                rope_evac(QT[:, p, tg * 512:(tg + 1) * 512], QT, psm, psr, cosH, sinH, (0, 128), tg,
                                          0.125, t1, t2)
                        late_init()
                        em.barrier()
                    for (off, dst, nm) in ((O_NKW, KwT, "kw"), (O_NKS, KsT, "ks")):
                        with ExitStack() as sp_:
                            wk1 = raw[nm]
                            wkf = S(sp_, "wnf" + nm, [128, 8, 256], BF16)
                            wkf4 = wkf[:].rearrange("p c (g f d) -> p c g f d", g=2, f=2)
                            for g in range(2):
                                for hf in range(2):
                                    cp("dve", wkf4[:, :, g, hf, :], wk1[:, :, g * 64:(g + 1) * 64], [wk1], [wkf])
                            wkr = make_rot(sp_, "wnr" + nm, wkf, 4, 32)
                            for tg in range(4):
                                for g in range(2):
                                    psm = PS.next()
                                    psr = PS.next()
                                    proj_fm(psm, wkf, g * 128, (g + 1) * 128, hT, tg)
                                    proj_fm(psr, wkr, g * 128, (g + 1) * 128, hT, tg)
                                    rope_evac(None, dst, psm, psr, cosH, sinH, (0, 128), tg, 1.0, t1, t2,
                                              dsts=[(0, 64, dst[0:64, g, 0, tg * 512:(tg + 1) * 512]),
                                                    (64, 128, dst[64:128, g, 1, tg * 512:(tg + 1) * 512])])
                            em.barrier()
                    with ExitStack() as sp_:
                        wkc = raw["kc"]
                        wkcr = make_rot(sp_, "wnkcr", wkc, 2, 32)
                        wvc = raw["vc"]
                        wtm = raw["tm"]
                        for tg in range(4):
                            psm = PS.next()
                            psr = PS.next()
                            proj_fm(psm, wkc, 0, 128, hT, tg)
                            proj_fm(psr, wkcr, 0, 128, hT, tg)
                            rope_evac(KcT[:, tg * 512:(tg + 1) * 512], KcT, psm, psr, cosH, sinH, (0, 128), tg, 1.0,
                                      t1, t2, extra=(cpt, cpt))
                            psm = PS.next()
                            proj_fm(psm, wvc, 0, 128, hT, tg)
                            tt("dve", VcT[:, tg * 512:(tg + 1) * 512], psm[:], cpt[:], ALU.add, [psm, cpt], [VcT])
                        for i in range(NT):
                            ps = PS.next()
                            for c in range(8):
                                mm(ps[:, 0:280], hT[:, c, i * 128:(i + 1) * 128], wtm[:, c, :], c == 0, c == 7,
                                   [hT, wtm], [ps])
                            cp("act", Vs[:, i, :, 0:64], ps[:, 0:128].rearrange("p (g d) -> p g d", d=64), [ps], [Vs])
                            cp("act", Vw[:, i, :, 0:64], ps[:, 128:256].rearrange("p (g d) -> p g d", d=64), [ps], [Vw])
                            act(gates[:, i, :], ps[:, 256:280], AF.Sigmoid, [ps], [gates])
                        em.barrier()
                    with ExitStack() as sp_:
                        phi2k = S(sp_, "phi2k", [128, 2, 128], BF16)
                        phi2v = S(sp_, "phi2v", [128, 2, 64], BF16)
                        hid = S(sp_, "hid", [128, 2, 128], BF16)
                        gx = S(sp_, "gx", [128, 128])
                        gu = S(sp_, "gu", [128, 128])
                        for hf in range(2):
                            load("pool", phi2k, phi2k[:, :, hf * 64:(hf + 1) * 64],
                                 phi_k2[l].rearrange("(c p) d -> p c d", p=128), ch)
                        load("pool", phi2v, phi2v[:], phi_v2[l].rearrange("(c p) d -> p c d", p=128), ch)
                        for which, src, phi in ((0, KcT, phi_k1), (1, VcT, phi_v1)):
                            phi1 = phi1s[which]
                            for hc in range(2):
                                for g in range(2):
                                    ps = PS.next()
                                    for i in range(32):
                                        mm(ps[:, 0:64], phi1[g * 64:(g + 1) * 64, i, hc * 128:(hc + 1) * 128],
                                           src[g * 64:(g + 1) * 64, i::32], i == 0, i == 31, [phi1, src], [ps])
                                    cp("act", gx[:, g * 64:(g + 1) * 64], ps[:, 0:64], [ps], [gx])
                                tt("dve", gu[:], gx[:], gx[:], ALU.mult, [gx], [gu])
                                ts("dve", gu[:], gu[:], 0.044715, 1.0, ALU.mult, ALU.add, [gu], [gu])
                                tt("dve", gu[:], gu[:], gx[:], ALU.mult, [gu, gx], [gu])
                                act(gu[:], gu[:], AF.Sigmoid, [gu], [gu], scale=1.5957691216057308)
                                tt("dve", hid[:, hc, :], gu[:], gx[:], ALU.mult, [gu, gx], [hid])
                            if which == 0:
                                ps = PS.next()
                                for hc in range(2):
                                    mm(ps[:, 0:128], phi2k[:, hc, :], hid[:, hc, :], hc == 0, hc == 1, [phi2k, hid], [ps])
                                cp("act", KcmpT[0:64, :, 0, :], ps[0:64, 0:128].rearrange("p (g c) -> p g c", c=64), [ps], [KcmpT])
                                cp("act", KcmpT[64:128, :, 1, :], ps[64:128, 0:128].rearrange("p (g c) -> p g c", c=64), [ps], [KcmpT])
                            else:
                                for g in range(2):
                                    ps = PS.next()
                                    for hc in range(2):
                                        mm(ps[0:64, 0:64], hid[:, hc, g * 64:(g + 1) * 64], phi2v[:, hc, :], hc == 0,
                                           hc == 1, [hid, phi2v], [ps])
                                    cp("act", Vcmp[0:64, g, :], ps[0:64, 0:64], [ps], [Vcmp])
                        em.barrier()
                    statq = S(sw, "statqn", [128, 16])
                    statkw = S(sw, "statkw", [128, 8])
                    statks = S(sw, "statks2", [128, 8])
                    sqn = Rot([S(sw, f"sqnn{i}", [128, 512], BF16) for i in range(3)])
                    for p in range(4):
                        for tg in range(4):
                            sumsq_max(QT[:, p, tg * 512:(tg + 1) * 512], QT, 128, statq, p * 4 + tg, sqn)
                    for g in range(2):
                        for tg in range(4):
                            sumsq_max(KwT[:, g, 0, tg * 512:(tg + 1) * 512], KwT, 128, statkw, g * 4 + tg, sqn)
                            sumsq_max(KsT[:, g, 0, tg * 512:(tg + 1) * 512], KsT, 128, statks, g * 4 + tg, sqn)
                    finish_negC(negCw, statq, 16, statkw, 8, 1.0, sw)
                    finish_negC(negCs, statq, 16, statks, 8, 1.0, sw)
                    em.barrier()
                selb_all = S(st, "selb_all", [128, NT, 2, 64], BF16)
                ocmp_all = S(st, "ocmp_all", [128, NT, 512], BF16)
                with ExitStack() as sA:
                    W = 4
                    mx_ = [S(sA, f"mx{k}", [128, 4]) for k in range(W)]
                    den_ = [S(sA, f"denn{k}", [128, 4]) for k in range(W)]
                    ee_ = [S(sA, f"ee{k}", [128, 4, 64]) for k in range(W)]
                    pp_ = [S(sA, f"ppn{k}", [128, 4, 64]) for k in range(W)]
                    pb_ = [S(sA, f"pbn{k}", [128, 4, 64], BF16) for k in range(W)]
                    pcT_ = [S(sA, f"pcT{k}", [128, 4, 128], BF16) for k in range(W)]
                    imp_ = [S(sA, f"imp{k}", [128, 64]) for k in range(W)]
                    max8_ = [S(sA, f"max8{k}", [128, 8]) for k in range(W)]
                    for k in range(W):
                        memset("pool", pcT_[k][:], 0.0, [pcT_[k]])

                    def genA(idx):
                        i, g = idx // 2, idx % 2
                        k = idx % W
                        mx, den, ee, pp, pb, pcT, imp, max8 = mx_[k], den_[k], ee_[k], pp_[k], pb_[k], pcT_[k], imp_[k], max8_[k]
                        psc = BK[2 * k]
                        mm(psc[:, 0:256], identb[:], cmpmask[:, i, :], True, False, [identb, cmpmask], [psc])
                        for sl in range(4):
                            hf, pl = sl // 2, sl % 2
                            mm(psc[:, sl * 64:(sl + 1) * 64], QT[:, 2 * g + pl, i * 128:(i + 1) * 128],
                               KcmpT[:, g, hf, :], False, sl == 3, [QT, KcmpT], [psc])
                        yield
                        red(mx[:], psc[:, 0:256].rearrange("p (s c) -> p s c", c=64), ALU.max, [psc], [mx])
                        ts("dve", mx[:], mx[:], -10000.0, -1.0, ALU.max, ALU.mult, [mx], [mx])
                        yield
                        for sl in range(4):
                            act(ee[:, sl, :], psc[:, sl * 64:(sl + 1) * 64], AF.Exp, [psc, mx], [ee, den],
                                bias=mx[:, sl:sl + 1], scale=1.0, accum=den[:, sl:sl + 1])
                        yield
                        ts("dve", den[:], den[:], 1e-30, None, ALU.max, None, [den], [den])
                        recip(den[:], den[:], [den], [den])
                        tt("dve", pp[:], ee[:], den[:].unsqueeze(2).to_broadcast([128, 4, 64]), ALU.mult, [ee, den], [pp])
                        cp("pool", pb[:], pp[:], [pp], [pb])
                        red(imp[:], pp[:].rearrange("p s c -> p c s"), ALU.add, [pp], [imp])
                        yield
                        psT = BfView(BK[2 * k + 1])
                        for sl in range(4):
                            tr(psT[0:64, sl, :], pb[:, sl, :], [pb, identb], [psT], identb[:])
                        yield
                        cp("act", pcT[0:64, :, :], psT[0:64, 0:4, :], [psT], [pcT])
                        tt("dve", imp[:], imp[:], keepM[:, i, :], ALU.mult, [imp, keepM], [imp])
                        tt("dve", imp[:], imp[:], addM[:, i, :], ALU.add, [imp, addM], [imp])
                        em.op("dve", lambda e: e.max(out=max8[:], in_=imp[:]), [imp], [max8])
                        yield
                        po = BK[2 * k + 1]
                        for sl in range(4):
                            mm(po[:, sl * 64:(sl + 1) * 64], pcT[:, sl, :], Vcmp[:, g, :], True, True, [pcT, Vcmp], [po])
                        ts("dve", imp[:], imp[:], max8[:, 7:8], None, ALU.is_ge, None, [imp, max8], [imp])
                        ts("dve", selb_all[:, i, g, :], imp[:], -NEG, NEG, ALU.mult, ALU.add, [imp], [selb_all])
                        yield
                        for sl in range(4):
                            hf, pl = sl // 2, sl % 2
                            h = 4 * g + 2 * pl + hf
                            act(ocmp_all[:, i, h * 64:(h + 1) * 64], po[:, sl * 64:(sl + 1) * 64], AF.Identity, [po, gates], [ocmp_all],
                                scale=gates[:, i, 3 * h:3 * h + 1])
                        yield
                    interleave([genA(idx) for idx in range(NT * 2)], W)
                    em.barrier()
                pT = Rot([S(st, f"pTn{i}", [128, 512], BF16) for i in range(4)])
                ot = Rot([S(st, f"otn{i}", [128, 512], BF16) for i in range(2)])
                och = [em.new_chan(16, "on") for _ in range(2)]
                nsel_r = Rot([S(st, f"nselT{i}", [128, 4, 128], BF16) for i in range(2)])
                for nt_ in nsel_r.items:
                    memset("pool", nt_[:], 0.0, [nt_])
                gr_r = Rot([S(st, f"grn{i}", [128, 1]) for i in range(4)])
                for i in range(NT):
                    o_t = ot.next()
                    for g in range(2):
                        nselT = nsel_r.next()
                        psT = PT.next()
                        tr(psT[0:64, 0, :], selb_all[:, i, g, :], [selb_all, identb], [psT], identb[:])
                        cp("act", nselT[0:64, :, :], psT[0:64, 0:1, :].to_broadcast([64, 4, 128]), [psT], [nselT])
                        po = PO.next()
                        mm(po[:, 0:260], zerosb[:, 0:128], zerosb[:, 0:260], True, False, [zerosb], [po])
                        pend = None
                        for j in range(i + 2):
                            cur = None
                            if j <= i:
                                ps = PS.next()
                                mm(ps[:], expandE[:, j * 128:(j + 1) * 128], nselT[:].rearrange("p s q -> p (s q)"), True,
                                   False, [expandE, nselT], [ps])
                                if j == i:
                                    mm(ps[:], identb[:], mdiag4[:], False, False, [identb, mdiag4], [ps])
                                for hf in range(2):
                                    mm(ps[:, hf * 256:(hf + 1) * 256].rearrange("p (a q) -> p a q", q=128),
                                       KsT[:, g, hf, j * 128:(j + 1) * 128],
                                       QT[:, 2 * g:2 * g + 2, i * 128:(i + 1) * 128], False, hf == 1,
                                       [KsT, QT], [ps])
                                p_t = pT.next()
                                act(p_t[:], ps[:], AF.Exp, [ps, negCs], [p_t], bias=negCs[:, 0:1], scale=1.0)
                                cur = (j, p_t)
                            if pend is not None:
                                jp, p_p = pend
                                for sl in range(4):
                                    mm(po[:, sl * 65:(sl + 1) * 65], p_p[:, sl * 128:(sl + 1) * 128], Vs[:, jp, g, :], False,
                                       (jp == i and sl == 3), [p_p, Vs], [po])
                            pend = cur
                        for sl in range(4):
                            hf, pl = sl // 2, sl % 2
                            h = 4 * g + 2 * pl + hf
                            gr = gr_r.next()
                            recip(gr[:], po[:, sl * 65 + 64:sl * 65 + 65], [po], [gr])
                            tt("dve", gr[:], gr[:], gates[:, i, 3 * h + 1:3 * h + 2], ALU.mult, [gr, gates], [gr])
                            stt(o_t[:, h * 64:(h + 1) * 64], po[:, sl * 65:sl * 65 + 64], gr[:, 0:1],
                                ocmp_all[:, i, h * 64:(h + 1) * 64], ALU.mult, ALU.add, [po, gr, ocmp_all], [o_t])
                        js = [i - 1, i] if i > 0 else [i]
                        pts = []
                        for j in js:
                            ps = PS.next()
                            msk = mdiag4 if j == i else mprev4
                            mm(ps[:], identb[:], msk[:], True, False, [identb, msk], [ps])
                            for hf in range(2):
                                mm(ps[:, hf * 256:(hf + 1) * 256].rearrange("p (a q) -> p a q", q=128),
                                   KwT[:, g, hf, j * 128:(j + 1) * 128],
                                   QT[:, 2 * g:2 * g + 2, i * 128:(i + 1) * 128], False, hf == 1,
                                   [KwT, QT], [ps])
                            p_t = pT.next()
                            act(p_t[:], ps[:], AF.Exp, [ps, negCw], [p_t], bias=negCw[:, 0:1], scale=1.0)
                            pts.append(p_t)
                        po = PO.next()
                        for sl in range(4):
                            for jj, j in enumerate(js):
                                mm(po[:, sl * 65:(sl + 1) * 65], pts[jj][:, sl * 128:(sl + 1) * 128], Vw[:, j, g, :],
                                   jj == 0, jj == len(js) - 1, [pts[jj], Vw], [po])
                        for sl in range(4):
                            hf, pl = sl // 2, sl % 2
                            h = 4 * g + 2 * pl + hf
                            gr = gr_r.next()
                            recip(gr[:], po[:, sl * 65 + 64:sl * 65 + 65], [po], [gr])
                            tt("dve", gr[:], gr[:], gates[:, i, 3 * h + 2:3 * h + 3], ALU.mult, [gr, gates], [gr])
                            stt(o_t[:, h * 64:(h + 1) * 64], po[:, sl * 65:sl * 65 + 64], gr[:, 0:1],
                                o_t[:, h * 64:(h + 1) * 64], ALU.mult, ALU.add, [po, gr, o_t], [o_t])
                    store("sp", o_scr[s, i * 128:(i + 1) * 128, 512:1024], dbuf(f"o{s}_{i}"), o_t, o_t[:], och[i % 2])
                em.barrier()

        def ln_affine(r, lng, lnb, xo, stats, mv, rstd, nb):
            for c2 in range(2):
                em.op("dve", lambda e, c2=c2: e.bn_stats(out=stats[:, c2, :], in_=r[:, c2 * 512:(c2 + 1) * 512]),
                      [r], [stats])
            em.op("dve", lambda e: e.bn_aggr(out=mv[:], in_=stats[:].rearrange("p a b -> p (a b)")), [stats], [mv])
            yield
            act(rstd[:], mv[:, 1:2], AF.Sqrt, [mv], [rstd], bias=LN_EPS, scale=1.0)
            yield
            recip(rstd[:], rstd[:], [rstd], [rstd])
            stt(nb[:], mv[:, 0:1], -1.0, rstd[:], ALU.mult, ALU.mult, [mv, rstd], [nb])
            yield
            act(xo[:], r[:], AF.Identity, [r, rstd, nb], [xo], bias=nb[:, 0:1], scale=rstd[:, 0:1])
            yield
            tt("dve", xo[:], xo[:], lng[:], ALU.mult, [xo, lng], [xo])
            yield
            tt("pool", xo[:], xo[:], lnb[:], ALU.add, [xo, lnb], [xo])
            yield

        def wout_stage(s, l, xsrc_ap, xsrc_buf, modT, h2T):
            with ExitStack() as st:
                ch = None
                wo = S(st, "wo", [128, 8, 1024], BF16)
                for c in range(8):
                    load("pool", wo, wo[:, c, :], w_out[l, c * 128:(c + 1) * 128, :], ch)
                G1 = S(st, "G1", [128, 1024])
                make_gate_bc(modT, 16, G1, st)
                lng = S(st, "lng1", [128, 1024])
                lnb = S(st, "lnb1", [128, 1024])
                load("sp", lng, lng[:], ln1_g[l:l + 1, :].to_broadcast([128, 1024]), ch)
                load("sp", lnb, lnb[:], ln1_b[l:l + 1, :].to_broadcast([128, 1024]), ch)
                W = 4
                otl = [S(st, f"otl{i}", [128, 1024], BF16) for i in range(W)]
                xt = [S(st, f"xtl{i}", [128, 1024]) for i in range(W)]
                lch = [em.new_chan(16, "wol") for _ in range(W)]
                lchx = [em.new_chan(16, "wolx") for _ in range(W)]
                oTt = [S(st, f"oTt{i}", [128, 8, 128], BF16) for i in range(W)]
                r_ = [S(st, f"r1{i}", [128, 1024]) for i in range(W)]
                xo = [S(st, f"xo{i}", [128, 1024]) for i in range(W)]
                sch = [em.new_chan(16, "wos") for _ in range(W)]
                stats_ = [S(st, f"stw{i}", [128, 2, 6]) for i in range(W)]
                mv_ = [S(st, f"mvw{i}", [128, 2]) for i in range(W)]
                rstd_ = [S(st, f"rstdw{i}", [128, 1]) for i in range(W)]
                nb_ = [S(st, f"nbw{i}", [128, 1]) for i in range(W)]
                hn_ = [S(st, f"hnw{i}", [128, 1024], BF16) for i in range(W)]

                def gen(i):
                    k = i % W
                    o_t, x_t, r, x_o = otl[k], xt[k], r_[k], xo[k]
                    load("sp", o_t, o_t[:], o_scr[s, i * 128:(i + 1) * 128, :], lch[k], src_buf=dbuf(f"o{s}_{i}"))
                    load("sp", x_t, x_t[:], xsrc_ap[i * 128:(i + 1) * 128, :], lchx[k], src_buf=dbuf(f"{xsrc_buf}_{i}"))
                    yield
                    psT = BfView(BK[2 * k])
                    for c in range(8):
                        tr(psT[:, c, :], o_t[:, c * 128:(c + 1) * 128], [o_t, identb], [psT], identb[:])
                    yield
                    cp("act", oTt[k][:], psT[:], [psT], [oTt[k]])
                    yield
                    for dh in range(2):
                        ps = BK[2 * k + 1]
                        for c in range(8):
                            mm(ps[:], oTt[k][:, c, :], wo[:, c, dh * 512:(dh + 1) * 512], c == 0, c == 7, [oTt[k], wo], [ps])
                        yield
                        tt("dve", r[:, dh * 512:(dh + 1) * 512], ps[:], G1[:, dh * 512:(dh + 1) * 512], ALU.mult,
                           [ps, G1], [r])
                        yield
                    stt(r[:], x_t[:], ALPHA, r[:], ALU.mult, ALU.add, [x_t, r], [r])
                    yield
                    yield from ln_affine(r, lng, lnb, x_o, stats_[k], mv_[k], rstd_[k], nb_[k])
                    store("sp", xa[s, i * 128:(i + 1) * 128, :], dbuf(f"xa{s}_{i}"), x_o, x_o[:], sch[k])
                    yield
                    yield from ln_tile_to_hT(x_o, modT, 24, 32, h2T, i, hn_[k], stats_[k], mv_[k], rstd_[k], psT)
                interleave([gen(i) for i in range(NT)], W)
                em.barrier()

        def moe_stage(s, l, modT, h2T, xdst_ap, xdst_buf):
            with ExitStack() as st:
                ch = None
                sele = S(st, "sele", [128, 2048], BF16)
                load("pool", sele, sele[:], k_sele, ch)
                rw = S(st, "rw", [128, 8, 16], BF16)
                load("pool", rw, rw[:], router_w.rearrange("(c p) n -> p c n", p=128), ch)
                rb = S(st, "rb", [128, 16])
                load("sp", rb, rb[:], router_bias.rearrange("(a n) -> a n", a=1).to_broadcast([128, 16]), ch)
                combT = S(st, "combT", [128, T_], BF16)
                memset("pool", combT[:], 0.0, [combT])
                yacc = S(st, "yacc", [128, NT, 1024])
                with ExitStack() as sr:
                    W = 4
                    mk = lambda nm, shp, dt=F32: [S(sr, f"{nm}{k}", shp, dt) for k in range(W)]
                    sc_, bi_, pr_, gsum_, gmax_, geq_ = mk("rsc", [128, 16]), mk("rbi", [128, 16]), mk("rpr", [128, 4, 6]), mk("rgs", [128, 4]), mk("rgm", [128, 1]), mk("rge", [128, 4])
                    m16_, mx8_, wsum_, cb_ = mk("rm16", [128, 16]), mk("rmx8", [128, 8]), mk("rws", [128, 1]), mk("rcb", [128, 16], BF16)

                    def genR(i):
                        k = i % W
                        sc, bi, pr, gsum, gmax, geq, m16, mx8, wsum, cb = sc_[k], bi_[k], pr_[k], gsum_[k], gmax_[k], geq_[k], m16_[k], mx8_[k], wsum_[k], cb_[k]
                        ps = BK[2 * k]
                        for c in range(8):
                            mm(ps[:, 0:16], h2T[:, c, i * 128:(i + 1) * 128], rw[:, c, :], c == 0, c == 7, [h2T, rw], [ps])
                        yield
                        act(sc[:], ps[:, 0:16], AF.Sigmoid, [ps], [sc])
                        yield
                        tt("dve", bi[:], sc[:], rb[:], ALU.add, [sc, rb], [bi])
                        b4 = bi[:].rearrange("p (g e) -> p g e", e=4)
                        tt("dve", pr[:, :, 0:3], b4[:, :, 0:3], b4[:, :, 1:4], ALU.add, [bi], [pr])
                        tt("dve", pr[:, :, 3:5], b4[:, :, 0:2], b4[:, :, 2:4], ALU.add, [bi], [pr])
                        tt("dve", pr[:, :, 5:6], b4[:, :, 0:1], b4[:, :, 3:4], ALU.add, [bi], [pr])
                        yield
                        red(gsum[:], pr[:], ALU.max, [pr], [gsum])
                        red(gmax[:], gsum[:], ALU.max, [gsum], [gmax])
                        yield
                        ts("dve", geq[:], gsum[:], gmax[:, 0:1], None, ALU.is_ge, None, [gsum, gmax], [geq])
                        ts("dve", geq[:], geq[:], 1e9, -1e9, ALU.mult, ALU.add, [geq], [geq])
                        tt("dve", m16[:].rearrange("p (g e) -> p g e", e=4), b4,
                           geq[:].unsqueeze(2).to_broadcast([128, 4, 4]), ALU.add, [bi, geq], [m16])
                        yield
                        em.op("dve", lambda e: e.max(out=mx8[:], in_=m16[:]), [m16], [mx8])
                        ts("dve", m16[:], m16[:], mx8[:, 1:2], None, ALU.is_ge, None, [m16, mx8], [m16])
                        tt("dve", m16[:], m16[:], sc[:], ALU.mult, [m16, sc], [m16])
                        yield
                        red(wsum[:], m16[:], ALU.add, [m16], [wsum])
                        recip(wsum[:], wsum[:], [wsum], [wsum])
                        ts("dve", cb[:], m16[:], wsum[:, 0:1], None, ALU.mult, None, [m16, wsum], [cb])
                        yield
                        psT = BfView(BK[2 * k + 1])
                        tr(psT[0:16, 0, :], cb[:], [cb, identb], [psT], identb[:])
                        yield
                        cp("act", combT[0:16, i * 128:(i + 1) * 128], psT[0:16, 0, :], [psT], [combT])
                        yield
                    interleave([genR(i) for i in range(NT)], W)
                    em.barrier()
                with ExitStack() as sx:
                    aT = S(sx, "aT", [128, 4, T_], BF16)
                    wgs = Rot([S(sx, f"wg{i}", [128, 8, 256], BF16) for i in range(2)])
                    wus = Rot([S(sx, f"wu{i}", [128, 8, 256], BF16) for i in range(2)])
                    wds = Rot([S(sx, f"wd{i}", [128, 2, 1024], BF16) for i in range(4)])
                    wchg = Rot([em.new_chan(16, "moewg") for _ in range(2)])
                    wchu = Rot([em.new_chan(16, "moewu") for _ in range(2)])
                    wchd = Rot([em.new_chan(16, "moewd") for _ in range(4)])
                    cbs = S(sx, "cbs", [128, 512], BF16)
                    sg = S(sx, "sg", [128, 512])
                    tu = S(sx, "tu", [128, 512])
                    for eg in range(8):
                        wd_g = []
                        for el in range(2):
                            e_ = eg * 2 + el
                            wg = wgs.next()
                            wu = wus.next()
                            wd = wds.next()
                            wcg = wchg.next()
                            wcu = wchu.next()
                            wcd = wchd.next()
                            for c4 in range(4):
                                load("pool", wg, wg[:, c4 * 2:c4 * 2 + 2, :],
                                     moe_wg[l, e_, c4 * 256:(c4 + 1) * 256, :].rearrange("(c p) n -> p c n", p=128), wcg)
                                load("pool", wu, wu[:, c4 * 2:c4 * 2 + 2, :],
                                     moe_wu[l, e_, c4 * 256:(c4 + 1) * 256, :].rearrange("(c p) n -> p c n", p=128), wcu)
                            load("pool", wd, wd[:], moe_wd[l, e_].rearrange("(c p) n -> p c n", p=128), wcd)
                            wd_g.append(wd)
                            for tg in range(4):
                                mm(psM[:], sele[:, e_ * 128:(e_ + 1) * 128], combT[:, tg * 512:(tg + 1) * 512], True, True,
                                   [sele, combT], [psM])
                                cp("act", cbs[:], psM[:], [psM], [cbs])
                                for fc in range(2):
                                    psg = PS.next()
                                    psu = PS.next()
                                    for c in range(8):
                                        mm(psg[:], wg[:, c, fc * 128:(fc + 1) * 128], h2T[:, c, tg * 512:(tg + 1) * 512],
                                           c == 0, c == 7, [wg, h2T], [psg])
                                    for c in range(8):
                                        mm(psu[:], wu[:, c, fc * 128:(fc + 1) * 128], h2T[:, c, tg * 512:(tg + 1) * 512],
                                           c == 0, c == 7, [wu, h2T], [psu])
                                    act(sg[:], psg[:], AF.Silu, [psg], [sg])
                                    tt("dve", tu[:], sg[:], psu[:], ALU.mult, [sg, psu], [tu])
                                    tt("dve", aT[:, el * 2 + fc, tg * 512:(tg + 1) * 512], tu[:], cbs[:], ALU.mult,
                                       [tu, cbs], [aT])
                        for i in range(NT):
                            for dh in range(2):
                                po = PO.next()
                                k = 0
                                for el in range(2):
                                    for fc in range(2):
                                        mm(po[:], aT[:, el * 2 + fc, i * 128:(i + 1) * 128],
                                           wd_g[el][:, fc, dh * 512:(dh + 1) * 512], k == 0, k == 3, [aT, wd_g[el]], [po])
                                        k += 1
                                if eg == 0:
                                    cp("act", yacc[:, i, dh * 512:(dh + 1) * 512], po[:], [po], [yacc])
                                else:
                                    tt("dve", yacc[:, i, dh * 512:(dh + 1) * 512], yacc[:, i, dh * 512:(dh + 1) * 512], po[:],
                                       ALU.add, [yacc, po], [yacc])
                    em.barrier()
                with ExitStack() as se:
                    G2 = S(se, "G2", [128, 1024])
                    make_gate_bc(modT, 40, G2, se)
                    lng = S(se, "lng2", [128, 1024])
                    lnb = S(se, "lnb2", [128, 1024])
                    load("sp", lng, lng[:], ln2_g[l:l + 1, :].to_broadcast([128, 1024]), ch)
                    load("sp", lnb, lnb[:], ln2_b[l:l + 1, :].to_broadcast([128, 1024]), ch)
                    W = 4
                    xt = [S(se, f"xte{i}", [128, 1024]) for i in range(W)]
                    lch = [em.new_chan(16, "mel") for _ in range(W)]
                    xo = [S(se, f"xoe{i}", [128, 1024]) for i in range(W)]
                    sch = [em.new_chan(16, "mes") for _ in range(W)]
                    r_ = [S(se, f"r2{i}", [128, 1024]) for i in range(W)]
                    stats_ = [S(se, f"ste{i}", [128, 2, 6]) for i in range(W)]
                    mv_ = [S(se, f"mve{i}", [128, 2]) for i in range(W)]
                    rstd_ = [S(se, f"rstde{i}", [128, 1]) for i in range(W)]
                    nb_ = [S(se, f"nbe{i}", [128, 1]) for i in range(W)]

                    def gen(i):
                        k = i % W
                        x_t, r, x_o = xt[k], r_[k], xo[k]
                        load("sp", x_t, x_t[:], xa[s, i * 128:(i + 1) * 128, :], lch[k], src_buf=dbuf(f"xa{s}_{i}"))
                        yield
                        tt("dve", r[:], yacc[:, i, :], G2[:], ALU.mult, [yacc, G2], [r])
                        yield
                        stt(r[:], x_t[:], ALPHA, r[:], ALU.mult, ALU.add, [x_t, r], [r])
                        yield
                        yield from ln_affine(r, lng, lnb, x_o, stats_[k], mv_[k], rstd_[k], nb_[k])
                        store("sp", xdst_ap[i * 128:(i + 1) * 128, :], dbuf(f"{xdst_buf}_{i}"), x_o, x_o[:], sch[k])
                        yield
                    interleave([gen(i) for i in range(NT)], W)
                    em.barrier()

        modA = S(gs, "modA", [128, n_layers, n_seq, 48])
        if upto >= 2:
            _tok = em.chan_mark()
            make_mod_all(modA)
            em.chan_release(_tok)
        for s in range(n_seq):
            _tok = em.chan_mark()
            make_tables(s)
            em.chan_release(_tok)
            for l in range(n_layers):
                if l == 0:
                    xsrc_ap, xsrc_buf = x_in[s], f"xin{s}"
                else:
                    xsrc_ap, xsrc_buf = xb[s], f"xb{s}"
                last = (l == n_layers - 1)
                xdst_ap, xdst_buf = (out[s], f"out{s}") if last else (xb[s], f"xb{s}")
                if upto < 2:
                    continue
                modT = ApView(modA[:, l, s, :], modA.b)
                if upto < 3:
                    continue
                with ExitStack() as sa:
                    hT = S(sa, "hT", [128, 8, T_], BF16)
                    with ExitStack() as sa2:
                        _tok = em.chan_mark()
                        ln_to_hT(xsrc_ap, xsrc_buf, modT, 0, 8, hT, sa2, "a")
                        em.barrier()
                        em.chan_release(_tok)
                    if upto >= 4:
                        _tok = em.chan_mark()
                        mla_stage(s, l, hT)
                        em.chan_release(_tok)
                    if upto >= 5:
                        _tok = em.chan_mark()
                        swa_stage(s, l, hT)
                        em.chan_release(_tok)
                    if upto >= 6:
                        _tok = em.chan_mark()
                        nsa_stage(s, l, hT)
                        em.chan_release(_tok)
                if upto < 7:
                    continue
                with ExitStack() as sb:
                    h2T = S(sb, "h2T", [128, 8, T_], BF16)
                    _tok = em.chan_mark()
                    wout_stage(s, l, xsrc_ap, xsrc_buf, modT, h2T)
                    em.chan_release(_tok)
                    if upto >= 8:
                        _tok = em.chan_mark()
                        moe_stage(s, l, modT, h2T, xdst_ap, xdst_buf)
                        em.chan_release(_tok)
            if dbg:
                dch = em.new_chan(16, "dbg")
                for i in range(NT):
                    em.dma("sp", dch, lambda e, s=s, i=i: e.dma_start(out=dbg_o[s, i * 128:(i + 1) * 128, :], in_=o_scr[s, i * 128:(i + 1) * 128, :]), [dbuf(f"o{s}_{i}")], [dbuf("dbgo")])
                    em.dma("sp", dch, lambda e, s=s, i=i: e.dma_start(out=dbg_x1[s, i * 128:(i + 1) * 128, :], in_=xa[s, i * 128:(i + 1) * 128, :]), [dbuf(f"xa{s}_{i}")], [dbuf("dbgx")])
        em.barrier()
        em.emit()
        print("ninst", em.ninst, "nsem", em.nsem, "n_inc", em.n_inc, flush=True)
    return nc


def make_consts():
    k = {}
    k["k_ident"] = np.eye(128, dtype=np.float32)
    p = np.arange(128)
    invf = np.ones((128, 2), np.float32)
    invf[:, 0] = (1.0 / (10000.0 ** ((2.0 * (p % 32)) / 64.0))).astype(np.float32)
    pm = p - 64
    invf[64:96, 1] = (1.0 / (10000.0 ** ((2.0 * (pm[64:96] % 16)) / 32.0))).astype(np.float32)
    k["k_invf"] = invf
    sgn = np.ones((128, 2), np.float32)
    sgn[:, 0] = np.where((p % 64) < 32, -1.0, 1.0)
    sgn[64:96, 1] = np.where(pm[64:96] < 16, -1.0, 1.0)
    k["k_sgn"] = sgn
    kk = np.arange(128)[:, None]
    qq = np.arange(128)[None, :]
    mdiag = np.where(kk <= qq, 0.0, NEG).astype(np.float32)
    mprev = np.where(kk > qq, 0.0, NEG).astype(np.float32)
    k["k_mdiag4"] = np.tile(mdiag, (1, 4))
    k["k_mprev4"] = np.tile(mprev, (1, 4))
    k["k_mwin4"] = np.concatenate([mprev, mprev, mdiag, mdiag], axis=1)
    t = (np.arange(NT)[None, :, None] * 128 + np.arange(128)[:, None, None])
    c = np.arange(64)[None, None, :]
    cm = np.where(c * 32 + 31 <= t, 0.0, NEG).astype(np.float32)
    k["k_cmpmask"] = np.tile(cm, (1, 1, 4)).reshape(128, NT * 256)
    tb = t // 32
    future = (c * 32 > t)
    forced = (c == 0) | (c == tb) | (c == tb - 1)
    keep = np.where(future | forced, 0.0, 1.0).astype(np.float32)
    add = np.where(future, -1e30, np.where(forced, 1e4, 0.0)).astype(np.float32)
    k["k_keep"] = keep.reshape(128, NT * 64)
    k["k_add"] = add.reshape(128, NT * 64)
    ex = np.zeros((128, 2048), np.float32)
    ex[0:64] = (np.arange(2048)[None, :] // 32 == np.arange(64)[:, None]).astype(np.float32)
    k["k_expand"] = ex
    se = np.zeros((128, 16, 128), np.float32)
    for e in range(16):
        se[e, e, :] = 1.0
    k["k_sele"] = se.reshape(128, 2048)
    return k


_NC_CACHE = {}


def kernel(**inputs):
    n_cores = 8
    if "nc" not in _NC_CACHE:
        _NC_CACHE["nc"] = build()
    nc = _NC_CACHE["nc"]
    consts = make_consts()
    shared = {k: np.ascontiguousarray(v) for k, v in inputs.items() if k not in ("x", "c", "positions")}
    in_maps = []
    for i in range(n_cores):
        m = dict(shared)
        m.update(consts)
        m["x"] = np.ascontiguousarray(inputs["x"][2 * i:2 * i + 2])
        m["c"] = np.ascontiguousarray(inputs["c"][2 * i:2 * i + 2])
        m["positions"] = np.ascontiguousarray(inputs["positions"][2 * i:2 * i + 2]).astype(np.int32)
        in_maps.append(m)
    res = run_bass_kernel_spmd(nc, in_maps, core_ids=list(range(n_cores)))
    return np.concatenate([r["out"] for r in res.results], axis=0).astype(np.float32)
```
